# Optimizing a Trainium2 kernel written in Bass

```python
import math
import jax, jax.numpy as jnp
from jax import lax
import numpy as np

D_MODEL = 2048
BATCH = 8
SEQ = 4096
DEPTH = 4

HEAD_DIM = 128
Q_BLOCK = 128
EPS = 1e-6
A_HEADS = D_MODEL // 512
A_WIDTH = A_HEADS * 2 * HEAD_DIM
B_HEADS = D_MODEL // 256
B_WIDTH = B_HEADS * HEAD_DIM
EVEN_SPLITS = (A_WIDTH, 2 * A_WIDTH, 3 * A_WIDTH, 3 * A_WIDTH + B_WIDTH, 3 * A_WIDTH + 2 * B_WIDTH)
EVEN_IN = 3 * A_WIDTH + 3 * B_WIDTH
EVEN_MIX = A_WIDTH + B_WIDTH
C_HEADS = D_MODEL // 256
C_WIDTH = C_HEADS * HEAD_DIM
DILATED = ((128, 1), (512, 4), (2048, 16))
D_CH = D_MODEL // 2
CONV_W = 31
ODD_SPLITS = (C_WIDTH, 2 * C_WIDTH, 3 * C_WIDTH)
ODD_IN = 3 * C_WIDTH + 2 * D_CH
ODD_MIX = C_WIDTH + D_CH
D_FF = ((8 * D_MODEL // 3 + 255) // 256) * 256
N_EXPERTS = 8
TOP_K = 2

kernel_name = "hybrid_diffattn_stickbreak_dilated_conformer_moe"


def rms_norm(x, g):
    xf = x.astype(jnp.float32)
    y = xf * lax.rsqrt(jnp.mean(xf * xf, axis=-1, keepdims=True) + EPS)
    return (y * g.astype(jnp.float32)).astype(x.dtype)


def layer_norm(x, g, b):
    xf = x.astype(jnp.float32)
    xc = xf - jnp.mean(xf, axis=-1, keepdims=True)
    y = xc * lax.rsqrt(jnp.mean(xc * xc, axis=-1, keepdims=True) + EPS)
    return (y * g.astype(jnp.float32) + b.astype(jnp.float32)).astype(x.dtype)


def alibi_slopes(n):
    return 2.0 ** (-8.0 * jnp.arange(1, n + 1, dtype=jnp.float32) / n)


def to_blocks(a):
    B, T = a.shape[:2]
    return a.reshape(B, T // Q_BLOCK, Q_BLOCK, *a.shape[2:]).swapaxes(0, 1)


def from_blocks(a):
    a = a.swapaxes(0, 1)
    return a.reshape(a.shape[0], a.shape[1] * a.shape[2], *a.shape[3:])


def differential_attention(q1, q2, k1, k2, v, lam, slopes):
    T, dh = q1.shape[1], q1.shape[-1]
    scale = dh ** -0.5
    kpos = jnp.arange(T)

    def one_block(args):
        i, qa, qb = args
        qpos = i * Q_BLOCK + jnp.arange(Q_BLOCK)
        dist = (qpos[:, None] - kpos[None, :]).astype(jnp.float32)
        bias = jnp.where(dist[None] >= 0, -slopes[:, None, None] * dist[None], -jnp.inf)

        def probs(qq, kk):
            s = jnp.einsum('bqhd,bkhd->bhqk', qq, kk).astype(jnp.float32) * scale + bias
            return jax.nn.softmax(s, axis=-1)

        p = probs(qa, k1) - lam * probs(qb, k2)
        return jnp.einsum('bhqk,bkhe->bqhe', p.astype(v.dtype), v)

    nb = T // Q_BLOCK
    out = lax.map(one_block, (jnp.arange(nb), to_blocks(q1), to_blocks(q2)))
    return from_blocks(out)


def stick_breaking_attention(q, k, v):
    T, dh = q.shape[1], q.shape[-1]
    scale = dh ** -0.5
    kpos = jnp.arange(T)

    def one_block(args):
        i, qb = args
        qpos = i * Q_BLOCK + jnp.arange(Q_BLOCK)
        past = kpos[None, :] < qpos[:, None]
        z = jnp.einsum('bqhd,bkhd->bhqk', qb, k).astype(jnp.float32) * scale
        log_beta = jax.nn.log_sigmoid(z)
        log_1m_beta = jnp.where(past, jax.nn.log_sigmoid(-z), 0.0)
        later = lax.cumsum(log_1m_beta, axis=3, reverse=True) - log_1m_beta
        w = jnp.where(past, jnp.exp(log_beta + later), 0.0)
        return jnp.einsum('bhqk,bkhd->bqhd', w.astype(v.dtype), v)

    nb = T // Q_BLOCK
    out = lax.map(one_block, (jnp.arange(nb), to_blocks(q)))
    return from_blocks(out)


def dilated_branch(q, k, v, window, dilation, slopes):
    B, T, H, dh = q.shape
    steps = window // dilation
    L = T // dilation
    nb = -(-L // Q_BLOCK)
    Lp = nb * Q_BLOCK

    def to_sub(a):
        e = a.shape[-1]
        a = a.reshape(B, L, dilation, H, e).transpose(0, 2, 1, 3, 4).reshape(B * dilation, L, H, e)
        return jnp.pad(a, ((0, 0), (0, Lp - L), (0, 0), (0, 0)))

    def band(a):
        X, e = a.shape[0], a.shape[-1]
        a = jnp.pad(a, ((0, 0), (Q_BLOCK, 0), (0, 0), (0, 0))).reshape(X, nb + 1, Q_BLOCK, H, e)
        return jnp.concatenate([a[:, :-1], a[:, 1:]], axis=2)

    def from_sub(a):
        e = a.shape[-1]
        a = a.reshape(B, dilation, Lp, H, e)[:, :, :L]
        return a.transpose(0, 2, 1, 3, 4).reshape(B, T, H, e)

    qb = to_sub(q).reshape(B * dilation, nb, Q_BLOCK, H, dh)
    kb, vb = band(to_sub(k)), band(to_sub(v))
    n_q = jnp.arange(nb)[:, None, None] * Q_BLOCK + jnp.arange(Q_BLOCK)[None, :, None]
    n_k = (jnp.arange(nb)[:, None, None] - 1) * Q_BLOCK + jnp.arange(2 * Q_BLOCK)[None, None, :]
    dist = n_q - n_k
    valid = (dist >= 0) & (dist <= steps) & (n_k >= 0)
    real_dist = (dist * dilation).astype(jnp.float32)
    bias = jnp.where(valid[:, None], -slopes[:, None, None] * real_dist[:, None], -jnp.inf)
    s = jnp.einsum('xnqhd,xnkhd->xnhqk', qb, kb).astype(jnp.float32) * dh ** -0.5 + bias
    m = jnp.max(s, axis=-1)
    p = jnp.exp(s - m[..., None])
    l = jnp.sum(p, axis=-1)
    acc = jnp.einsum('xnhqk,xnkhe->xnqhe', p.astype(v.dtype), vb).astype(jnp.float32)
    m = from_sub(m.transpose(0, 1, 3, 2)[..., None])
    l = from_sub(l.transpose(0, 1, 3, 2)[..., None])
    return m, l, from_sub(acc)


def dilated_mixture(q, k, v, slopes):
    parts = [dilated_branch(q, k, v, w, d, slopes) for (w, d) in DILATED]
    m_all = jnp.max(jnp.stack([p[0] for p in parts]), axis=0)
    num = jnp.zeros(q.shape, jnp.float32)
    den = jnp.zeros(q.shape[:-1] + (1,), jnp.float32)
    for m, l, acc in parts:
        r = jnp.exp(m - m_all)
        num = num + acc * r
        den = den + l * r
    return (num / den).astype(q.dtype)


def conformer_conv(u, conv_w, conv_b, ln_g, ln_b):
    a, g = jnp.split(u, 2, axis=-1)
    h = a * jax.nn.sigmoid(g)
    h = lax.conv_general_dilated(h, conv_w[:, None, :].astype(h.dtype), window_strides=(1,),
                                 padding=[(CONV_W - 1, 0)], dimension_numbers=('NWC', 'WIO', 'NWC'),
                                 feature_group_count=D_CH) + conv_b
    return jax.nn.silu(layer_norm(h, ln_g, ln_b))


def even_mixer(h, w_in, w_out, q_g, k_g, lam_vecs, head_g, lam_init):
    B, T, _ = h.shape
    aq, ak, av, bq, bk, bv = jnp.split(h @ w_in, EVEN_SPLITS, axis=-1)
    aq = aq.reshape(B, T, A_HEADS, 2, HEAD_DIM)
    ak = ak.reshape(B, T, A_HEADS, 2, HEAD_DIM)
    q1, q2 = rms_norm(aq[..., 0, :], q_g), rms_norm(aq[..., 1, :], q_g)
    k1, k2 = rms_norm(ak[..., 0, :], k_g), rms_norm(ak[..., 1, :], k_g)
    lv = lam_vecs.astype(jnp.float32)
    lam = jnp.exp(jnp.sum(lv[0] * lv[1])) - jnp.exp(jnp.sum(lv[2] * lv[3])) + lam_init
    oa = differential_attention(q1, q2, k1, k2, av.reshape(B, T, A_HEADS, 2 * HEAD_DIM), lam,
                                alibi_slopes(A_HEADS))
    oa = rms_norm(oa, head_g) * (1.0 - lam_init)
    shp = (B, T, B_HEADS, HEAD_DIM)
    ob = stick_breaking_attention(bq.reshape(shp), bk.reshape(shp), bv.reshape(shp))
    mixed = jnp.concatenate([oa.reshape(B, T, A_WIDTH), ob.reshape(B, T, B_WIDTH)], axis=-1)
    return mixed @ w_out


def odd_mixer(h, w_in, w_out, q_g, k_g, conv_w, conv_b, ln_g, ln_b):
    B, T, _ = h.shape
    cq, ck, cv, du = jnp.split(h @ w_in, ODD_SPLITS, axis=-1)
    shp = (B, T, C_HEADS, HEAD_DIM)
    oc = dilated_mixture(rms_norm(cq.reshape(shp), q_g), rms_norm(ck.reshape(shp), k_g),
                         cv.reshape(shp), alibi_slopes(C_HEADS))
    od = conformer_conv(du, conv_w, conv_b, ln_g, ln_b)
    mixed = jnp.concatenate([oc.reshape(B, T, C_WIDTH), od], axis=-1)
    return mixed @ w_out


def swiglu(h, wg, wu, wd):
    return (jax.nn.silu(h @ wg) * (h @ wu)) @ wd


def moe_swiglu(h, router_w, wg, wu, wd):
    B, T, D = h.shape
    xt = h.reshape(B * T, D)
    logits = (xt @ router_w).astype(jnp.float32)
    top_vals, top_idx = lax.top_k(logits, TOP_K)
    top_w = jax.nn.softmax(top_vals, axis=-1)
    gates = jnp.sum(jax.nn.one_hot(top_idx, N_EXPERTS, dtype=jnp.float32) * top_w[..., None], axis=1)
    y = jnp.zeros_like(xt)
    for e in range(N_EXPERTS):
        y = y + gates[:, e:e + 1].astype(xt.dtype) * swiglu(xt, wg[e], wu[e], wd[e])
    return y.reshape(B, T, D)


def setup_inputs(seed: int = 0) -> dict:
    key = jax.random.key(seed)
    keys = iter(jax.random.split(key, 32))
    D = D_MODEL
    NE = (DEPTH + 1) // 2
    NO = DEPTH // 2

    def nrm(shape, s):
        return jax.random.normal(next(keys), shape, jnp.float32) * s

    return {
        "x": nrm((BATCH, SEQ, D), 1.0),
        "c": nrm((BATCH, D), 1.0),
        "ada_w": nrm((DEPTH, D, 6 * D), 0.5 * D ** -0.5),
        "ada_b": nrm((DEPTH, 6 * D), 0.01),
        "norm_mix_g": 1.0 + nrm((DEPTH, D), 0.02),
        "norm_ffn_g": 1.0 + nrm((DEPTH, D), 0.02),
        "ev_w_in": nrm((NE, D, EVEN_IN), D ** -0.5),
        "ev_w_out": nrm((NE, EVEN_MIX, D), EVEN_MIX ** -0.5),
        "a_q_norm_g": 1.0 + nrm((NE, HEAD_DIM), 0.02),
        "a_k_norm_g": 1.0 + nrm((NE, HEAD_DIM), 0.02),
        "a_lambda": nrm((NE, 4, HEAD_DIM), 0.1),
        "a_head_norm_g": 1.0 + nrm((NE, 2 * HEAD_DIM), 0.02),
        "ffn_w_gate": nrm((NE, D, D_FF), D ** -0.5),
        "ffn_w_up": nrm((NE, D, D_FF), D ** -0.5),
        "ffn_w_down": nrm((NE, D_FF, D), D_FF ** -0.5),
        "od_w_in": nrm((NO, D, ODD_IN), D ** -0.5),
        "od_w_out": nrm((NO, ODD_MIX, D), ODD_MIX ** -0.5),
        "c_q_norm_g": 1.0 + nrm((NO, HEAD_DIM), 0.02),
        "c_k_norm_g": 1.0 + nrm((NO, HEAD_DIM), 0.02),
        "d_conv_w": nrm((NO, CONV_W, D_CH), CONV_W ** -0.5),
        "d_conv_b": nrm((NO, D_CH), 0.01),
        "d_ln_g": 1.0 + nrm((NO, D_CH), 0.02),
        "d_ln_b": nrm((NO, D_CH), 0.01),
        "moe_router": nrm((NO, D, N_EXPERTS), D ** -0.5),
        "moe_w_gate": nrm((NO, N_EXPERTS, D, D_FF), D ** -0.5),
        "moe_w_up": nrm((NO, N_EXPERTS, D, D_FF), D ** -0.5),
        "moe_w_down": nrm((NO, N_EXPERTS, D_FF, D), D_FF ** -0.5),
    }


def reference(x, c, ada_w, ada_b, norm_mix_g, norm_ffn_g, ev_w_in, ev_w_out, a_q_norm_g,
              a_k_norm_g, a_lambda, a_head_norm_g, ffn_w_gate, ffn_w_up, ffn_w_down, od_w_in,
              od_w_out, c_q_norm_g, c_k_norm_g, d_conv_w, d_conv_b, d_ln_g, d_ln_b, moe_router,
              moe_w_gate, moe_w_up, moe_w_down):
    cond = jax.nn.silu(c)
    for i in range(DEPTH):
        j = i // 2
        ada = (cond @ ada_w[i] + ada_b[i])[:, None, :]
        sh1, sc1, g1, sh2, sc2, g2 = jnp.split(ada, 6, axis=-1)
        h = rms_norm(x, norm_mix_g[i]) * (1.0 + sc1) + sh1
        if i % 2 == 0:
            lam_init = 0.8 - 0.6 * math.exp(-0.3 * i)
            mix = even_mixer(h, ev_w_in[j], ev_w_out[j], a_q_norm_g[j], a_k_norm_g[j],
                             a_lambda[j], a_head_norm_g[j], lam_init)
        else:
            mix = odd_mixer(h, od_w_in[j], od_w_out[j], c_q_norm_g[j], c_k_norm_g[j],
                            d_conv_w[j], d_conv_b[j], d_ln_g[j], d_ln_b[j])
        x = x + g1 * mix
        h = rms_norm(x, norm_ffn_g[i]) * (1.0 + sc2) + sh2
        if i % 2 == 0:
            ff = swiglu(h, ffn_w_gate[j], ffn_w_up[j], ffn_w_down[j])
        else:
            ff = moe_swiglu(h, moe_router[j], moe_w_gate[j], moe_w_up[j], moe_w_down[j])
        x = x + g2 * ff
    return x
```

```python
import contextlib
import math
import numpy as np
import ml_dtypes
import concourse.bass as bass
import concourse.mybir as mybir
from concourse.bass_utils import run_bass_kernel_spmd

F32 = mybir.dt.float32
BF16 = mybir.dt.bfloat16
AF = mybir.ActivationFunctionType
ALU = mybir.AluOpType
PE, ACT, DVE, POOL, SP = "pe", "act", "dve", "pool", "sp"
P = 128
TT = 512
EPS = 1e-6
NEG = -30000.0
import os as _os
EMBED_WAIT = _os.environ.get("KDBG_EMBED", "1") == "1"
CONV_PE = _os.environ.get("KDBG_CONVPE", "1") == "1"


class Cfg:
    def __init__(self, D=2048, T=4096, NB=1, L=4, NE=8, ncores=8):
        self.D, self.T, self.NB, self.L, self.NE, self.ncores = D, T, NB, L, NE, ncores
        self.KC = D // P
        self.AH = D // 512
        self.AW = self.AH * 256
        self.BH = D // 256
        self.BW = self.BH * 128
        self.EVEN_IN = 3 * self.AW + 3 * self.BW
        self.CH = D // 256
        self.CW = self.CH * 128
        self.DCH = D // 2
        self.ODD_IN = 3 * self.CW + 2 * self.DCH
        self.DFF = ((8 * D // 3 + 255) // 256) * 256
        self.KF = self.DFF // P
        self.NEV = (L + 1) // 2
        self.NOD = L // 2
        self.NQT = T // TT
        self.NCK = T // P


class Tl:
    __slots__ = ("w", "r", "dsem")

    def __init__(self):
        self.w = None
        self.r = {}
        self.dsem = None


class Sched:
    def __init__(self, nc, st):
        self.nc = nc
        self.st = st
        self.ops = {e: [] for e in (PE, ACT, DVE, POOL, SP)}
        self.cnt = {e: 0 for e in self.ops}
        self.seen = {e: {} for e in self.ops}
        self.dcount = {}
        self.nd = 0
        self.sems = {}

    def t(self):
        return Tl()

    def _deps(self, eng, reads, writes, pe_accum):
        need = {}
        for t in reads:
            if t.w is not None and need.get(t.w[0], 0) < t.w[1]:
                need[t.w[0]] = t.w[1]
        for t in writes:
            if t.w is not None and not (pe_accum and t.w[0] == PE and eng == PE):
                if need.get(t.w[0], 0) < t.w[1]:
                    need[t.w[0]] = t.w[1]
            for k, v in t.r.items():
                if need.get(k, 0) < v:
                    need[k] = v
        waits = []
        seen = self.seen[eng]
        for k, v in need.items():
            if k == PE and eng == PE:
                continue
            if seen.get(k, 0) >= v:
                continue
            seen[k] = v
            waits.append((k, v))
        return waits

    def op(self, eng, fn, reads=(), writes=(), pe_accum=False):
        waits = self._deps(eng, reads, writes, pe_accum)
        self.cnt[eng] += 1
        c = self.cnt[eng]
        self.ops[eng].append((waits, fn, (eng, 1)))
        for t in reads:
            t.r[eng] = c
        for t in writes:
            t.w = (eng, c)
            t.r = {}

    def dma(self, q, out_ap, in_ap, own, reads=(), writes=()):
        if own.dsem is None:
            own.dsem = ("d", self.nd)
            self.nd += 1
            self.dcount.setdefault(own.dsem, 0)
        waits = self._deps(q, reads, writes, False)
        self.dcount[own.dsem] += 16
        v = self.dcount[own.dsem]
        self.ops[q].append((waits, lambda e: e.dma_start(out=out_ap, in_=in_ap), (own.dsem, 16)))
        for t in reads:
            t.r[own.dsem] = v
        for t in writes:
            t.w = (own.dsem, v)
            t.r = {}

    def _sem(self, k):
        if k not in self.sems:
            self.sems[k] = self.st.enter_context(self.nc.semaphore("s_%s" % (k if isinstance(k, str) else "d%d" % k[1])))
        return self.sems[k]

    def end_phase(self, label="ph"):
        import os
        self.nphase = getattr(self, "nphase", 0) + 1
        lim = int(os.environ.get("KDBG_STOP", "0"))
        if lim and self.nphase > lim:
            for e in self.ops:
                self.ops[e] = []
            self.nd = 0
            return
        for e in self.ops:
            waits = []
            for k in (PE, ACT, DVE, POOL):
                if k != e and self.cnt[k] > self.seen[e].get(k, 0):
                    waits.append((k, self.cnt[k]))
                    self.seen[e][k] = self.cnt[k]
            for k, v in self.dcount.items():
                if v > self.seen[e].get(k, 0):
                    waits.append((k, v))
                    self.seen[e][k] = v
            if waits:
                self.ops[e].append((waits, None, None))
        nc = self.nc
        with nc.named_scope("p%03d_%s" % (self.nphase, label)), nc.Block() as block:
            def run(name):
                def body(eobj):
                    for waits, fn, inc in self.ops[name]:
                        if fn is None or not EMBED_WAIT or not waits:
                            for k, v in waits:
                                eobj.wait_ge(self._sem(k), v)
                            if fn is not None:
                                fn(eobj).then_inc(self._sem(inc[0]), inc[1])
                        else:
                            for k, v in waits[1:]:
                                eobj.wait_ge(self._sem(k), v)
                            ins = fn(eobj)
                            ins._wait_ge(self._sem(waits[0][0]), waits[0][1])
                            ins.then_inc(self._sem(inc[0]), inc[1])
                return body
            block.tensor(run(PE))
            block.scalar(run(ACT))
            block.vector(run(DVE))
            block.gpsimd(run(POOL))
            block.sync(run(SP))
        st = getattr(self, 'stats', {})
        for e in self.ops:
            st[e] = st.get(e, 0) + sum(1 for w, f, i in self.ops[e] if f is not None)
            st['w_' + e] = st.get('w_' + e, 0) + sum(len(w) for w, f, i in self.ops[e])
        self.stats = st
        for e in self.ops:
            self.ops[e] = []
        self.nd = 0

    def mm(self, out_t, out_ap, l_t, l_ap, r_t, r_ap, start, stop):
        self.op(PE, lambda e: e.matmul(out_ap, lhsT=l_ap, rhs=r_ap, start=start, stop=stop), [l_t, r_t], [out_t], pe_accum=True)

    def tr(self, out_t, out_ap, i_t, i_ap, id_t, id_ap):
        self.op(PE, lambda e: e.transpose(out=out_ap, in_=i_ap, identity=id_ap), [i_t, id_t], [out_t])

    def act(self, out_t, out_ap, in_t, in_ap, func, bias=None, scale=None, extra_reads=(), eng=ACT):
        kw = {}
        if bias is not None:
            kw["bias"] = bias
        if scale is not None:
            kw["scale"] = scale
        self.op(eng, lambda e: e.activation(out=out_ap, in_=in_ap, func=func, **kw), [in_t] + list(extra_reads), [out_t])

    def tt(self, eng, out_t, out_ap, a_t, a_ap, b_t, b_ap, op):
        self.op(eng, lambda e: e.tensor_tensor(out=out_ap, in0=a_ap, in1=b_ap, op=op), [a_t, b_t], [out_t])

    def ts(self, eng, out_t, out_ap, a_t, a_ap, s1, s2, op0, op1=None, extra_reads=()):
        if op1 is None:
            self.op(eng, lambda e: e.tensor_scalar(out=out_ap, in0=a_ap, scalar1=s1, scalar2=None, op0=op0), [a_t] + list(extra_reads), [out_t])
        else:
            self.op(eng, lambda e: e.tensor_scalar(out=out_ap, in0=a_ap, scalar1=s1, scalar2=s2, op0=op0, op1=op1), [a_t] + list(extra_reads), [out_t])

    def stt(self, out_t, out_ap, a_t, a_ap, scalar, b_t, b_ap, op0, op1, extra_reads=()):
        self.op(DVE, lambda e: e.scalar_tensor_tensor(out=out_ap, in0=a_ap, scalar=scalar, in1=b_ap, op0=op0, op1=op1), [a_t, b_t] + list(extra_reads), [out_t])

    def cp(self, eng, out_t, out_ap, in_t, in_ap):
        if eng == ACT:
            self.op(ACT, lambda e: e.activation(out=out_ap, in_=in_ap, func=AF.Copy), [in_t], [out_t])
        else:
            self.op(eng, lambda e: e.tensor_copy(out=out_ap, in_=in_ap), [in_t], [out_t])

    def ms(self, eng, out_t, out_ap, val):
        self.op(eng, lambda e: e.memset(out_ap, val), [], [out_t])


def alibi(n):
    return [2.0 ** (-8.0 * (h + 1) / n) for h in range(n)]


def make_consts(cfg):
    scale = 128 ** -0.5
    j = np.arange(P)[:, None].astype(np.float64)
    i = np.arange(TT)[None, :].astype(np.float64)
    cb = {}
    cb["ident"] = np.eye(P)
    cb["ones"] = np.ones((P, P))
    cb["onesD"] = np.full((P, P), 1.0 / cfg.D)
    cb["ones128"] = np.full((P, P), 1.0 / 128)
    cb["onesDCH"] = np.full((P, P), 1.0 / cfg.DCH)
    s = np.arange(P)[None, :]
    cb["tri"] = (np.arange(P)[:, None] >= s).astype(np.float64)
    for d in range(4):
        cb["mneg%d" % d] = np.where(i >= 128 * d + j, 0.0, NEG)

    def count(dl):
        return ((dl >= 0) & (dl <= 128)).astype(np.float64) + ((dl >= 0) & (dl <= 512) & (dl % 4 == 0)) + \
            ((dl >= 0) & (dl <= 2048) & (dl % 16 == 0))
    for u in range(20):
        d0 = 128 * (u - 3)
        cb["cnt%d" % u] = count((d0 + i - j).astype(np.int64))
    slc = alibi(cfg.CH)
    for h in range(cfg.CH):
        cb["shift%d" % h] = np.broadcast_to(-(slc[h] / scale) * (i - 256.0), (P, TT))
    cf = {}
    cf["identf"] = np.eye(P)
    cf["onesf"] = np.ones((P, P))
    for d in range(4):
        cf["m01%d" % d] = (i > 128 * d + j).astype(np.float64)
    sla = alibi(cfg.AH)
    dc = np.arange(-31, 4)[None, :]
    for h in range(cfg.AH):
        cf["biasA%d" % h] = sla[h] * (128.0 * dc + j - 256.0)
    dc2 = np.arange(-16, 4)[None, :]
    for h in range(cfg.CH):
        cf["biasC%d" % h] = slc[h] * (128.0 * dc2 + j - 256.0)

    def pack(dct, dt):
        offs, cols, o = {}, [], 0
        for k, v in dct.items():
            v = np.asarray(v)
            offs[k] = (o, v.shape[1])
            o += v.shape[1]
            cols.append(v)
        return offs, np.ascontiguousarray(np.concatenate(cols, axis=1).astype(np.float32)).astype(dt)
    cb2 = {k: cb.pop(k) for k in list(cb) if k.startswith("cnt") or k.startswith("shift")}
    ob, ab = pack(cb, ml_dtypes.bfloat16)
    ob2, ab2 = pack(cb2, ml_dtypes.bfloat16)
    of, af = pack(cf, np.float32)
    return ob, ab, of, af, ob2, ab2


def build_program(cfg, layers=None):
    if layers is None:
        layers = list(range(cfg.L))
    D, T, NB, KC, KF, NE, DFF = cfg.D, cfg.T, cfg.NB, cfg.KC, cfg.KF, cfg.NE, cfg.DFF
    NQT, NCK = cfg.NQT, cfg.NCK
    ob, ab, of, af, ob2, ab2 = make_consts(cfg)
    nc = bass.Bass("TRN2", target_bir_lowering=False)

    def din(name, shape, dt=F32):
        return nc.dram_tensor(name, list(shape), dt, kind="ExternalInput").ap()

    def dscr(name, shape, dt):
        return nc.dram_tensor(name, list(shape), dt, kind="Internal").ap()

    L, NEV, NOD = cfg.L, cfg.NEV, cfg.NOD
    xT_in = din("xT", [NB, D, T])
    cT_in = din("cT", [P, NB * KC])
    ada_w = din("ada_w", [L, D, 6 * D])
    ada_bT = din("ada_bT", [L, P, 6 * KC])
    nmgT = din("nmgT", [L, P, KC])
    nfgT = din("nfgT", [L, P, KC])
    ev_w_in = din("ev_w_in", [NEV, D, cfg.EVEN_IN])
    ev_w_out = din("ev_w_out", [NEV, D, D])
    aqg = din("aqg", [NEV, P, 1])
    akg = din("akg", [NEV, P, 1])
    alam = din("alam", [NEV, P, 4])
    ahg = din("ahg", [NEV, P, 256])
    ffn_wg = din("ffn_wg", [NEV, D, DFF])
    ffn_wu = din("ffn_wu", [NEV, D, DFF])
    ffn_wd = din("ffn_wd", [NEV, DFF, D])
    od_w_in = din("od_w_in", [NOD, D, cfg.ODD_IN])
    od_w_out = din("od_w_out", [NOD, D, D])
    cqg = din("cqg", [NOD, P, 1])
    ckg = din("ckg", [NOD, P, 1])
    KD = cfg.DCH // P
    convw = din("convw", [NOD, P, KD * 31])
    convb = din("convb", [NOD, P, KD])
    lng = din("lng", [NOD, P, KD])
    lnb = din("lnb", [NOD, P, KD])
    routT = din("routT", [NOD, P, KC * NE])
    moe_wg = din("moe_wg", [NOD, NE, D, DFF])
    moe_wu = din("moe_wu", [NOD, NE, D, DFF])
    moe_wd = din("moe_wd", [NOD, NE, DFF, D])
    cb_in = din("cb", list(ab.shape), BF16)
    cf_in = din("cf", list(af.shape), F32)
    cb2_in = din("cb2", list(ab2.shape), BF16)
    yT_out = nc.dram_tensor("yT", [NB, D, T], F32, kind="ExternalOutput").ap()

    xres = dscr("xres", [NB, D, T], F32)
    xb = dscr("xb", [D, T], F32)
    xc = dscr("xc", [D, T], F32)
    hT = dscr("hT", [D, T], BF16)
    QKR = max(2 * cfg.AW + 2 * cfg.BW, 2 * cfg.CW + 2 * cfg.DCH)
    qkT = dscr("qkT", [QKR, T], BF16)
    VW = max(cfg.AW + cfg.BW, cfg.CW)
    vtm = dscr("vtm", [T, VW], BF16)
    mixT = dscr("mixT", [D, T], BF16)
    AT = dscr("AT", [DFF, T], BF16)
    gatesB = dscr("gatesB", [NE, P, T], F32)
    ycv = dscr("ycv", [cfg.DCH, T], F32)

    scale = 128 ** -0.5

    with contextlib.ExitStack() as top:
        S = Sched(nc, top)

        uid = [0]
        LBL = ["lin"]

        def sbt(st, name, shape, dt):
            uid[0] += 1
            return st.enter_context(nc.sbuf_tensor("%s_u%d" % (name, uid[0]), list(shape), dt))

        def pst_(st, name, shape, dt):
            uid[0] += 1
            return st.enter_context(nc.psum_tensor("%s_u%d" % (name, uid[0]), list(shape), dt))

        cbs = sbt(top, "cbs", ab.shape, BF16)
        cfs = sbt(top, "cfs", af.shape, F32)
        condT = sbt(top, "condT", [P, NB * KC], F32)
        adaT = sbt(top, "adaT", [P, NB * 6 * KC], F32)
        colA = sbt(top, "colA", [P, NB * 2 * KC], F32)
        smallp = sbt(top, "smallp", [P, 2 * KC + 16 + 256 + KD * 34 + KC * NE], F32)
        t_const = S.t()
        t_cond = S.t()
        t_ada = S.t()
        t_colA = S.t()
        t_small = S.t()

        def CB(name, rows=slice(0, P)):
            o, w = ob[name]
            return cbs[rows, o:o + w]

        def CF(name, rows=slice(0, P)):
            o, w = of[name]
            return cfs[rows, o:o + w]

        o_ng = 0
        o_misc = 2 * KC
        o_hg = o_misc + 16
        o_cw = o_hg + 256
        o_rt = o_cw + KD * 34

        S.dma(SP, cbs[:], cb_in, t_const, writes=[t_const])
        S.dma(SP, cfs[:], cf_in, t_const, writes=[t_const])
        S.dma(SP, condT[:], cT_in, t_cond, writes=[t_cond])
        S.act(t_cond, condT[:], t_cond, condT[:], AF.Silu)
        S.end_phase(LBL[0] if "init" == "linear" else "init")

        def phase_ada(l):
            with contextlib.ExitStack() as st:
                NBLK = 6 * D // TT
                wt = [sbt(st, "adaw%d" % i, [P, KC, TT], F32) for i in range(2)]
                wtt = [S.t() for _ in range(2)]
                row = sbt(st, "adarow", [1, NB * 6 * D], F32)
                t_row = S.t()
                bT = sbt(st, "adab", [P, 6 * KC], F32)
                t_b = S.t()
                ng = sbt(st, "ng", [P, 2 * KC], F32)
                t_ng = S.t()
                ps = pst_(st, "ps_ada", [P, 4, TT], F32)
                pst = [S.t() for _ in range(4)]
                S.dma(SP, bT[:], ada_bT[l], t_b, writes=[t_b])
                S.dma(SP, ng[:, 0:KC], nmgT[l], t_ng, writes=[t_ng])
                S.dma(SP, ng[:, KC:2 * KC], nfgT[l], t_ng, writes=[t_ng])
                wsrc = ada_w[l].rearrange("(kc p) n -> p kc n", p=P)

                def ld(i):
                    S.dma(SP if i % 2 == 0 else ACT, wt[i % 2][:], wsrc[:, :, i * TT:(i + 1) * TT], wtt[i % 2], writes=[wtt[i % 2]])
                ld(0)
                k = 0
                for i in range(NBLK):
                    if i + 1 < NBLK:
                        ld(i + 1)
                    for b in range(NB):
                        pb = k % 3
                        k += 1
                        for kc in range(KC):
                            S.mm(pst[pb], ps[0:1, pb, :], t_cond, condT[:, b * KC + kc:b * KC + kc + 1],
                                 wtt[i % 2], wt[i % 2][:, kc, :], kc == 0, kc == KC - 1)
                        S.cp(ACT, t_row, row[0:1, b * 6 * D + i * TT: b * 6 * D + (i + 1) * TT], pst[pb], ps[0:1, pb, :])
                for b in range(NB):
                    for jn in range(6 * KC):
                        S.mm(pst[3], ps[:, 3, b * 6 * KC + jn: b * 6 * KC + jn + 1], t_row,
                             row[0:1, b * 6 * D + jn * P: b * 6 * D + (jn + 1) * P], t_const, CF("onesf", slice(0, 1))[:, 0:1], True, True)
                for b in range(NB):
                    S.tt(DVE, t_ada, adaT[:, b * 6 * KC:(b + 1) * 6 * KC], pst[3], ps[:, 3, b * 6 * KC:(b + 1) * 6 * KC], t_b, bT[:], ALU.add)
                    for which, (osc, og) in enumerate(((KC, 0), (4 * KC, KC))):
                        dst = colA[:, (b * 2 + which) * KC:(b * 2 + which + 1) * KC]
                        S.ts(DVE, t_colA, dst, t_ada, adaT[:, b * 6 * KC + osc: b * 6 * KC + osc + KC], 1.0, None, ALU.add)
                        S.tt(DVE, t_colA, dst, t_colA, dst, t_ng, ng[:, og:og + KC], ALU.mult)
                S.end_phase(LBL[0] if "ada" == "linear" else "ada")

        def ada_col(b, which):
            return adaT[:, b * 6 * KC + which * KC: b * 6 * KC + (which + 1) * KC]

        def phase_norm(b, x_src, sub, router_l=None):
            Acol = colA[:, (b * 2 + sub) * KC:(b * 2 + sub + 1) * KC]
            Bcol = ada_col(b, 0 if sub == 0 else 3)
            TN = 256
            with contextlib.ExitStack() as st:
                xt = [sbt(st, "nx%d" % i, [P, KC, TN], F32) for i in range(2)]
                xtt = [S.t() for _ in range(2)]
                sq = sbt(st, "nsq", [P, KC, TN], BF16)
                t_sq = S.t()
                hb = [sbt(st, "nhb%d" % i, [P, KC, TN], BF16) for i in range(2)]
                hbt = [S.t() for _ in range(2)]
                rstd = sbt(st, "nrstd", [P, TN], F32)
                t_rstd = S.t()
                tmp = [sbt(st, "ntmp%d" % i, [P, TN], F32) for i in range(2)]
                tmpt = [S.t() for _ in range(2)]
                ps = pst_(st, "ps_n", [P, 4, TT], F32)
                pst = [S.t() for _ in range(4)]
                if router_l is not None:
                    rwf = sbt(st, "nrwf", [P, KC * NE], F32)
                    t_rwf = S.t()
                    S.dma(SP, rwf[:], routT[router_l], t_rwf, writes=[t_rwf])
                    rw = sbt(st, "nrw", [P, KC * NE], BF16)
                    t_rw = S.t()
                    S.cp(DVE, t_rw, rw[:], t_rwf, rwf[:])
                    lg = sbt(st, "nlg", [P, 8 * NE], F32)
                    t_lg = S.t()
                    dg = [sbt(st, "ndg%d" % i, [P, P], BF16) for i in range(2)]
                    dgt = [S.t() for _ in range(2)]
                    gb = [sbt(st, "ngb%d" % i, [P, NE, TN], F32) for i in range(2)]
                    gbt = [S.t() for _ in range(2)]
                xsrc = x_src.rearrange("(kc p) t -> p kc t", p=P)
                hdst = hT.rearrange("(kc p) t -> p kc t", p=P)

                def ld(i):
                    S.dma(SP, xt[i % 2][:], xsrc[:, :, i * TN:(i + 1) * TN], xtt[i % 2], writes=[xtt[i % 2]])
                ld(0)
                for i in range(T // TN):
                    if i + 1 < T // TN:
                        ld(i + 1)
                    X, tX = xt[i % 2], xtt[i % 2]
                    H, tH = hb[i % 2], hbt[i % 2]
                    S.act(t_sq, sq[:], tX, X[:], AF.Square)
                    for kc in range(KC):
                        S.mm(pst[0], ps[:, 0, 0:TN], t_const, CB("onesD"), t_sq, sq[:, kc, :], kc == 0, kc == KC - 1)
                    S.act(t_rstd, rstd[:], pst[0], ps[:, 0, 0:TN], AF.Ln, bias=EPS)
                    S.act(t_rstd, rstd[:], t_rstd, rstd[:], AF.Exp, scale=-0.5)
                    for kc in range(KC):
                        tm, ttm = tmp[kc % 2], tmpt[kc % 2]
                        S.stt(ttm, tm[:], tX, X[:, kc, :], Acol[:, kc:kc + 1], t_rstd, rstd[:], ALU.mult, ALU.mult, extra_reads=[t_colA])
                        S.act(tH, H[:, kc, :], ttm, tm[:], AF.Identity, bias=Bcol[:, kc:kc + 1], extra_reads=[t_ada])
                    S.dma(SP, hdst[:, :, i * TN:(i + 1) * TN], H[:], tH, reads=[tH])
                    if router_l is not None:
                        G, tG = gb[i % 2], gbt[i % 2]
                        for s in range(TN // P):
                            pl = ps[:, 1, s * NE:(s + 1) * NE]
                            for kc in range(KC):
                                S.mm(pst[1], pl, tH, H[:, kc, s * P:(s + 1) * P], t_rw, rw[:, kc * NE:(kc + 1) * NE], kc == 0, kc == KC - 1)
                            lgs = lg[:, 0:NE]
                            m1 = lg[:, NE:NE + 1]
                            eq = lg[:, 2 * NE:3 * NE]
                            l2 = lg[:, 3 * NE:4 * NE]
                            m2 = lg[:, 4 * NE:4 * NE + 1]
                            ex = lg[:, 5 * NE:6 * NE]
                            den = lg[:, 6 * NE:6 * NE + 1]
                            gt = lg[:, 7 * NE:8 * NE]
                            S.cp(DVE, t_lg, lgs, pst[1], pl)
                            S.op(DVE, lambda e, a=m1, b_=lgs: e.reduce_max(out=a, in_=b_, axis=mybir.AxisListType.X), [t_lg], [t_lg])
                            S.ts(DVE, t_lg, eq, t_lg, lgs, m1, None, ALU.is_ge)
                            S.stt(t_lg, l2, t_lg, eq, -10000.0, t_lg, lgs, ALU.mult, ALU.add)
                            S.op(DVE, lambda e, a=m2, b_=l2: e.reduce_max(out=a, in_=b_, axis=mybir.AxisListType.X), [t_lg], [t_lg])
                            S.ts(DVE, t_lg, eq, t_lg, lgs, m2, None, ALU.is_ge)
                            S.ts(DVE, t_lg, ex, t_lg, lgs, m1, None, ALU.subtract)
                            S.act(t_lg, ex, t_lg, ex, AF.Exp)
                            S.tt(DVE, t_lg, ex, t_lg, ex, t_lg, eq, ALU.mult)
                            S.op(DVE, lambda e, a=den, b_=ex: e.reduce_sum(out=a, in_=b_, axis=mybir.AxisListType.X), [t_lg], [t_lg])
                            S.op(DVE, lambda e, a=den: e.reciprocal(out=a, in_=a), [t_lg], [t_lg])
                            S.ts(DVE, t_lg, gt, t_lg, ex, den, None, ALU.mult)
                            for ex_ in range(NE):
                                dd, tdd = dg[ex_ % 2], dgt[ex_ % 2]
                                S.ts(DVE, tdd, dd[:], t_const, CB("ident"), gt[:, ex_:ex_ + 1], None, ALU.mult, extra_reads=[t_lg])
                                pb = 2 + (ex_ % 2)
                                S.mm(pst[pb], ps[:, pb, 0:P], t_const, CB("ones"), tdd, dd[:], True, True)
                                S.cp(ACT, tG, G[:, ex_, s * P:(s + 1) * P], pst[pb], ps[:, pb, 0:P])
                        S.dma(SP, gatesB.rearrange("e p t -> p e t")[:, :, i * TN:(i + 1) * TN], G[:], tG, reads=[tG])
                S.end_phase(LBL[0] if "norm" == "linear" else "norm")

        def phase_linear(x_dram, K, Ws, N, mode, **kw):
            KCk = K // P
            LBL[0] = "lin_%s_K%d_N%d" % (mode, K, N)
            import os
            kct = int(os.environ.get("KDBG_KCT", "16"))
            nbw = TT if KCk <= kct else 256
            TX = TT if KCk <= kct else 256
            NQX = T // TX
            if N % nbw:
                nbw = 256
            assert N % nbw == 0
            NBK = N // nbw
            NJ = nbw // P
            nW = len(Ws)
            with contextlib.ExitStack() as st:
                wb = [[sbt(st, "lw%d_%d" % (w, i), [P, KCk, nbw], BF16) for i in range(2)] for w in range(nW)]
                wbt = [[S.t() for _ in range(2)] for _ in range(nW)]
                xt = [sbt(st, "lx%d" % i, [P, KCk, TX], BF16) for i in range(3)]
                xtt = [S.t() for _ in range(3)]
                ps = pst_(st, "ps_l", [P, 8, TT], F32)
                pst = [S.t() for _ in range(8)]
                xsrc = x_dram.rearrange("(kc p) t -> p kc t", p=P)
                wsrc = [W.rearrange("(kc p) n -> p kc n", p=P) for W in Ws]
                if mode in ("plain", "qknorm", "glu"):
                    og = [sbt(st, "lo%d" % i, [P, NJ, TX], BF16) for i in range(2)]
                    ogt = [S.t() for _ in range(2)]
                if mode == "qknorm":
                    sq = [sbt(st, "lsq%d" % i, [P, TX], BF16) for i in range(2)]
                    sqt = [S.t() for _ in range(2)]
                    rs = [sbt(st, "lrs%d" % i, [P, TX], F32) for i in range(2)]
                    rst = [S.t() for _ in range(2)]
                if mode == "glu":
                    sg = [sbt(st, "lsg%d" % i, [P, TX], F32) for i in range(2)]
                    sgt = [S.t() for _ in range(2)]
                    if kw.get("gate_e") is not None:
                        gbx = [sbt(st, "lgb%d" % i, [P, TX], F32) for i in range(3)]
                        gbxt = [S.t() for _ in range(3)]
                if mode == "tm":
                    ot = [sbt(st, "lot%d" % i, [P, TX // P, nbw], BF16) for i in range(2)]
                    ott = [S.t() for _ in range(2)]
                if mode == "resid":
                    xi = [sbt(st, "lxi%d" % i, [P, NJ, TX], F32) for i in range(3)]
                    xit = [S.t() for _ in range(3)]
                    xo = [sbt(st, "lxo%d" % i, [P, NJ, TX], F32) for i in range(2)]
                    xot = [S.t() for _ in range(2)]
                    rsrc = kw["x_src"].rearrange("(c p) t -> p c t", p=P)
                    rdst = kw["x_dst"].rearrange("(c p) t -> p c t", p=P)

                items = [(nb_, ti) for nb_ in range(NBK) for ti in range(NQX)]

                def ldw(nb_):
                    for w in range(nW):
                        S.dma(POOL, wb[w][nb_ % 2][:], wsrc[w][:, :, nb_ * nbw:(nb_ + 1) * nbw], wbt[w][nb_ % 2], writes=[wbt[w][nb_ % 2]])

                def ldx(ix):
                    nb_, ti = items[ix]
                    S.dma(SP, xt[ix % 3][:], xsrc[:, :, ti * TX:(ti + 1) * TX], xtt[ix % 3], writes=[xtt[ix % 3]])
                    if mode == "resid":
                        S.dma(SP, xi[ix % 3][:], rsrc[:, nb_ * NJ:(nb_ + 1) * NJ, ti * TX:(ti + 1) * TX], xit[ix % 3], writes=[xit[ix % 3]])
                    if mode == "glu" and kw.get("gate_e") is not None:
                        S.dma(SP, gbx[ix % 3][:], gatesB[kw["gate_e"], :, ti * TX:(ti + 1) * TX], gbxt[ix % 3], writes=[gbxt[ix % 3]])
                ldw(0)
                ldx(0)
                if len(items) > 1:
                    ldx(1)
                pk = 0
                for ix, (nb_, ti) in enumerate(items):
                    if ti == 0 and nb_ + 1 < NBK:
                        ldw(nb_ + 1)
                    if ix + 2 < len(items):
                        ldx(ix + 2)
                    X, tX = xt[ix % 3], xtt[ix % 3]
                    Wb = [wb[w][nb_ % 2] for w in range(nW)]
                    tW = [wbt[w][nb_ % 2] for w in range(nW)]
                    if mode == "tm":
                        O, tO = ot[ix % 2], ott[ix % 2]
                        for s in range(TX // P):
                            pb = pk % 8
                            pk += 1
                            for kc in range(KCk):
                                S.mm(pst[pb], ps[:, pb, 0:nbw], tX, X[:, kc, s * P:(s + 1) * P], tW[0], Wb[0][:, kc, :], kc == 0, kc == KCk - 1)
                            S.cp(ACT if s % 2 == 0 else DVE, tO, O[:, s, :], pst[pb], ps[:, pb, 0:nbw])
                        c0 = kw["dst_col"] + nb_ * nbw
                        S.dma(SP, vtm[ti * TX:(ti + 1) * TX, c0:c0 + nbw].rearrange("(s p) n -> p s n", p=P), O[:], tO, reads=[tO])
                        continue
                    if mode in ("plain", "qknorm", "glu"):
                        O, tO = og[ix % 2], ogt[ix % 2]
                    if mode == "resid":
                        O, tO = xo[ix % 2], xot[ix % 2]
                    for jn in range(NJ):
                        pbs = []
                        for w in range(nW):
                            pb = pk % 6
                            pk += 1
                            pbs.append(pb)
                            for kc in range(KCk):
                                S.mm(pst[pb], ps[:, pb, 0:TX], tW[w], Wb[w][:, kc, jn * P:(jn + 1) * P], tX, X[:, kc, :], kc == 0, kc == KCk - 1)
                        pb = pbs[0]
                        if mode == "plain":
                            S.cp(ACT if jn % 2 == 0 else DVE, tO, O[:, jn, :], pst[pb], ps[:, pb, 0:TX])
                        elif mode == "qknorm":
                            q2, tq2 = sq[jn % 2], sqt[jn % 2]
                            r2, tr2 = rs[jn % 2], rst[jn % 2]
                            S.act(tq2, q2[:], pst[pb], ps[:, pb, 0:TX], AF.Square)
                            pb2 = 6 + (jn % 2)
                            S.mm(pst[pb2], ps[:, pb2, 0:TX], t_const, CB("ones128"), tq2, q2[:], True, True)
                            S.act(tr2, r2[:], pst[pb2], ps[:, pb2, 0:TX], AF.Ln, bias=EPS)
                            S.act(tr2, r2[:], tr2, r2[:], AF.Exp, scale=-0.5)
                            S.stt(tO, O[:, jn, :], pst[pb], ps[:, pb, 0:TX], kw["gcol"], tr2, r2[:], ALU.mult, ALU.mult, extra_reads=[t_small])
                        elif mode == "glu":
                            g2, tg2 = sg[jn % 2], sgt[jn % 2]
                            S.act(tg2, g2[:], pst[pbs[0]], ps[:, pbs[0], 0:TX], AF.Silu)
                            if kw.get("gate_e") is not None:
                                S.tt(DVE, tg2, g2[:], tg2, g2[:], pst[pbs[1]], ps[:, pbs[1], 0:TX], ALU.mult)
                                S.tt(POOL, tO, O[:, jn, :], tg2, g2[:], gbxt[ix % 3], gbx[ix % 3][:], ALU.mult)
                            else:
                                S.tt(DVE, tO, O[:, jn, :], tg2, g2[:], pst[pbs[1]], ps[:, pbs[1], 0:TX], ALU.mult)
                        elif mode == "resid":
                            cj = nb_ * NJ + jn
                            S.stt(tO, O[:, jn, :], pst[pb], ps[:, pb, 0:TX], kw["gcol"][:, cj:cj + 1], xit[ix % 3], xi[ix % 3][:, jn, :],
                                  ALU.mult, ALU.add, extra_reads=[t_ada])
                    if mode == "resid":
                        S.dma(SP, rdst[:, nb_ * NJ:(nb_ + 1) * NJ, ti * TX:(ti + 1) * TX], O[:], tO, reads=[tO])
                    else:
                        r0 = kw["dst_row"] + nb_ * nbw
                        dd = kw["dst"][r0:r0 + nbw, ti * TX:(ti + 1) * TX].rearrange("(c p) t -> p c t", p=P)
                        S.dma(SP, dd, O[:], tO, reads=[tO])
                S.end_phase(LBL[0] if "linear" == "linear" else "linear")

        def phase_down(x_dram, K, W, N, x_src, x_dst, gcol):
            KCk = K // P
            assert KCk % 2 == 0
            KH = KCk // 2
            nbw, NJ = 256, 2
            NBK = N // nbw
            LBL[0] = "down_K%d_N%d" % (K, N)
            with contextlib.ExitStack() as st:
                wb = [sbt(st, "dw%d" % i, [P, KCk, nbw], BF16) for i in range(2)]
                wbt = [S.t() for _ in range(2)]
                xt = [sbt(st, "dx%d" % i, [P, KH, TT], BF16) for i in range(3)]
                xtt = [S.t() for _ in range(3)]
                xi = [sbt(st, "dxi%d" % i, [P, NJ, TT], F32) for i in range(3)]
                xit = [S.t() for _ in range(3)]
                xo = [sbt(st, "dxo%d" % i, [P, NJ, TT], F32) for i in range(2)]
                xot = [S.t() for _ in range(2)]
                ps = pst_(st, "ps_d", [P, 8, TT], F32)
                pst = [S.t() for _ in range(8)]
                xsrc = x_dram.rearrange("(kc p) t -> p kc t", p=P)
                wsrc = W.rearrange("(kc p) n -> p kc n", p=P)
                rsrc = x_src.rearrange("(c p) t -> p c t", p=P)
                rdst = x_dst.rearrange("(c p) t -> p c t", p=P)
                parts = [(nb_, ti, hf) for nb_ in range(NBK) for ti in range(NQT) for hf in range(2)]

                def ldw(nb_):
                    S.dma(POOL, wb[nb_ % 2][:], wsrc[:, :, nb_ * nbw:(nb_ + 1) * nbw], wbt[nb_ % 2], writes=[wbt[nb_ % 2]])

                def ldx(px):
                    nb_, ti, hf = parts[px]
                    S.dma(SP, xt[px % 3][:], xsrc[:, hf * KH:(hf + 1) * KH, ti * TT:(ti + 1) * TT], xtt[px % 3], writes=[xtt[px % 3]])
                    if hf == 0:
                        ix = px // 2
                        S.dma(SP, xi[ix % 3][:], rsrc[:, nb_ * NJ:(nb_ + 1) * NJ, ti * TT:(ti + 1) * TT], xit[ix % 3], writes=[xit[ix % 3]])
                ldw(0)
                ldx(0)
                ldx(1)
                for px, (nb_, ti, hf) in enumerate(parts):
                    ix = px // 2
                    if ti == 0 and hf == 0 and nb_ + 1 < NBK:
                        ldw(nb_ + 1)
                    if px + 2 < len(parts):
                        ldx(px + 2)
                    X, tX = xt[px % 3], xtt[px % 3]
                    Wb, tW = wb[nb_ % 2], wbt[nb_ % 2]
                    for jn in range(NJ):
                        pb = (ix % 4) * 2 + jn
                        for k in range(KH):
                            kc = hf * KH + k
                            S.mm(pst[pb], ps[:, pb, :], tW, Wb[:, kc, jn * P:(jn + 1) * P], tX, X[:, k, :], kc == 0, kc == KCk - 1)
                    if hf == 1:
                        O, tO = xo[ix % 2], xot[ix % 2]
                        for jn in range(NJ):
                            pb = (ix % 4) * 2 + jn
                            cj = nb_ * NJ + jn
                            S.stt(tO, O[:, jn, :], pst[pb], ps[:, pb, :], gcol[:, cj:cj + 1], xit[ix % 3], xi[ix % 3][:, jn, :],
                                  ALU.mult, ALU.add, extra_reads=[t_ada])
                        S.dma(SP, rdst[:, nb_ * NJ:(nb_ + 1) * NJ, ti * TT:(ti + 1) * TT], O[:], tO, reads=[tO])
                S.end_phase(LBL[0])

        def phase_softattn(kind, l_idx):
            if kind == "diff":
                NH, EW = cfg.AH, 256
                lam_init = 0.8 - 0.6 * math.exp(-0.3 * l_idx)
            else:
                NH, EW = cfg.CH, 128
            EA = EW + 1
            with contextlib.ExitStack() as st:
                nmap = 2 if kind == "diff" else 1
                kq = [[sbt(st, "ak%d_%d" % (m, i), [P, 2, T], BF16) for i in range(2)] for m in range(nmap)]
                kqt = [[S.t() for _ in range(2)] for _ in range(nmap)]
                va = [sbt(st, "av%d" % i, [P, NCK, EA], BF16) for i in range(2)]
                vat = [S.t() for _ in range(2)]
                pT = [sbt(st, "ap%d" % i, [P, TT], BF16) for i in range(3)]
                pTt = [S.t() for _ in range(3)]
                if kind == "dil":
                    pe_ = [sbt(st, "ape%d" % i, [P, TT], BF16) for i in range(2)]
                    pet = [S.t() for _ in range(2)]
                on = sbt(st, "aon", [P, 2, 4, EW], F32)
                t_on = [[S.t() for _ in range(4)] for _ in range(2)]
                rl = sbt(st, "arl", [P, 16], F32)
                t_rl = S.t()
                ob_ = [sbt(st, "aob%d" % i, [P, EW], BF16) for i in range(2)]
                obt = [S.t() for _ in range(2)]
                jk = sbt(st, "ajk", [P, EW], F32)
                t_jk = S.t()
                mo = [sbt(st, "amo%d" % i, [P, EW // P, TT], BF16) for i in range(2)]
                mot = [S.t() for _ in range(2)]
                ps = pst_(st, "ps_a", [P, 7, TT], F32)
                pst = [S.t() for _ in range(7)]
                psb = pst_(st, "ps_ab", [P, 4, P], BF16)
                psbt = [S.t()] * 4
                if kind == "dil":
                    cb2s = sbt(st, "cb2s", ab2.shape, BF16)
                    t_c2 = S.t()
                    S.dma(SP, cb2s[:], cb2_in, t_c2, writes=[t_c2])

                    def CB2(name, rows=slice(0, P)):
                        o, w = ob2[name]
                        return cb2s[rows, o:o + w]
                misc = smallp[:, o_misc:o_misc + 16]
                hg = smallp[:, o_hg:o_hg + 256]
                if kind == "diff":
                    j = l_idx // 2
                    S.dma(SP, misc[:, 0:4], alam[j], t_small, writes=[t_small])
                    S.dma(SP, hg, ahg[j], t_small, writes=[t_small])
                    S.tt(DVE, t_small, misc[:, 4:5], t_small, misc[:, 0:1], t_small, misc[:, 1:2], ALU.mult)
                    S.tt(DVE, t_small, misc[:, 5:6], t_small, misc[:, 2:3], t_small, misc[:, 3:4], ALU.mult)
                    S.mm(pst[6], ps[:, 6, 0:2], t_const, CF("onesf"), t_small, misc[:, 4:6], True, True)
                    S.act(t_small, misc[:, 6:8], pst[6], ps[:, 6, 0:2], AF.Exp)
                    S.tt(DVE, t_small, misc[:, 8:9], t_small, misc[:, 6:7], t_small, misc[:, 7:8], ALU.subtract)
                    S.ts(DVE, t_small, misc[:, 9:10], t_small, misc[:, 8:9], lam_init, -1.0, ALU.add, ALU.mult)
                    S.ts(DVE, t_small, hg, t_small, hg, 1.0 - lam_init, None, ALU.mult)
                    neglam = misc[:, 9:10]

                def load_head(h):
                    i = h % 2
                    if kind == "diff":
                        rows = [(cfg.AW + (2 * h + m) * P, (2 * h + m) * P) for m in range(2)]
                        vcol = h * 256
                    else:
                        rows = [(cfg.CW + h * P, h * P)]
                        vcol = h * P
                    for m in range(nmap):
                        S.dma(SP, kq[m][i][:, 0, :], qkT[rows[m][0]:rows[m][0] + P, :], kqt[m][i], writes=[kqt[m][i]])
                        S.dma(SP, kq[m][i][:, 1, :], qkT[rows[m][1]:rows[m][1] + P, :], kqt[m][i], writes=[kqt[m][i]])
                    S.dma(SP, va[i][:, :, 0:EW], vtm[:, vcol:vcol + EW].rearrange("(c p) e -> p c e", p=P), vat[i], writes=[vat[i]])
                    S.ms(POOL, vat[i], va[i][:, :, EW:EA], 1.0)
                load_head(0)
                pT.append(sbt(st, "ap3", [P, TT], BF16))
                pTt.append(S.t())
                if kind == "dil":
                    pe_.append(sbt(st, "ape2", [P, TT], BF16))
                    pet.append(S.t())
                LA = 2
                for h in range(NH):
                    if h + 1 < NH:
                        load_head(h + 1)
                    i = h % 2
                    V, tV = va[i], vat[i]
                    jobs = []
                    for qt in range(NQT):
                        for m in range(nmap):
                            clo = 0 if kind == "diff" else max(0, 4 * qt - 16)
                            chi = 4 * qt + 3
                            for c in range(clo, chi + 1):
                                jobs.append((qt, m, c, c == chi))

                    def emit_S(ix):
                        qt, m, c, _ = jobs[ix]
                        KQ, tKQ = kq[m][i], kqt[m][i]
                        pb = 4 + (ix % 3)
                        d = c - 4 * qt
                        if kind == "diff":
                            S.mm(pst[pb], ps[:, pb, :], tKQ, KQ[:, 0, c * P:(c + 1) * P], tKQ, KQ[:, 1, qt * TT:(qt + 1) * TT], True, d < 0)
                            if d >= 0:
                                S.mm(pst[pb], ps[:, pb, :], t_const, CB("ident"), t_const, CB("mneg%d" % d), False, True)
                        else:
                            S.mm(pst[pb], ps[:, pb, :], tKQ, KQ[:, 0, c * P:(c + 1) * P], tKQ, KQ[:, 1, qt * TT:(qt + 1) * TT], True, False)
                            S.mm(pst[pb], ps[:, pb, :], t_const, CB("ones", slice(0, 1)), t_c2, CB2("shift%d" % h, slice(0, 1)), False, True)
                    for ix in range(min(LA, len(jobs))):
                        emit_S(ix)
                    for ix, (qt, m, c, lastc) in enumerate(jobs):
                        if ix + LA < len(jobs):
                            emit_S(ix + LA)
                        MO, tMO = mo[qt % 2], mot[qt % 2]
                        pb = 4 + (ix % 3)
                        d = c - 4 * qt
                        Pt, tPt = pT[ix % 4], pTt[ix % 4]
                        if kind == "diff":
                            o_, w_ = of["biasA%d" % h]
                            S.act(tPt, Pt[:], pst[pb], ps[:, pb, :], AF.Exp, bias=cfs[:, o_ + d + 31:o_ + d + 32], scale=scale, extra_reads=[t_const])
                        else:
                            Pe, tPe = pe_[ix % 3], pet[ix % 3]
                            o_, w_ = of["biasC%d" % h]
                            S.act(tPe, Pe[:], pst[pb], ps[:, pb, :], AF.Exp, bias=cfs[:, o_ + d + 16:o_ + d + 17], scale=scale, extra_reads=[t_const])
                            u = (4 * qt - c) + 3
                            S.tt(DVE if ix % 2 else POOL, tPt, Pt[:], tPe, Pe[:], t_c2, CB2("cnt%d" % u), ALU.mult)
                        for s in range(4):
                            cs = 4 * qt + s
                            if kind == "diff":
                                lo, hi = 0, cs
                            else:
                                lo, hi = max(0, cs - 16), cs
                            if c < lo or c > hi:
                                continue
                            S.mm(pst[s], ps[:, s, 0:EA], tPt, Pt[:, s * P:(s + 1) * P], tV, V[:, c, :], c == lo, c == hi)
                        if not lastc:
                            continue
                        for s in range(4):
                            S.op(DVE, lambda e, a=rl[:, m * 4 + s:m * 4 + s + 1], b_=ps[:, s, EW:EA]: e.reciprocal(out=a, in_=b_), [pst[s]], [t_rl])
                            S.ts(DVE, t_on[m][s], on[:, m, s, :], pst[s], ps[:, s, 0:EW], rl[:, m * 4 + s:m * 4 + s + 1], None, ALU.mult, extra_reads=[t_rl])
                        if m != nmap - 1:
                            continue
                        for s in range(4):
                            OB, tOB = ob_[s % 2], obt[s % 2]
                            if kind == "diff":
                                S.stt(t_on[0][s], on[:, 0, s, :], t_on[1][s], on[:, 1, s, :], neglam, t_on[0][s], on[:, 0, s, :], ALU.mult, ALU.add, extra_reads=[t_small])
                                ss = rl[:, 8 + s:9 + s]
                                S.ms(DVE, t_rl, ss, 0.0)
                                S.op(ACT, lambda e, o1=jk[:], i1=on[:, 0, s, :], a1=ss: e.activation(out=o1, in_=i1, func=AF.Square, accum_out=a1), [t_on[0][s]], [t_jk, t_rl])
                                S.act(t_rl, ss, t_rl, ss, AF.Ln, bias=EPS, scale=1.0 / 256)
                                S.act(t_rl, ss, t_rl, ss, AF.Exp, scale=-0.5)
                                S.stt(tOB, OB[:], t_on[0][s], on[:, 0, s, :], ss, t_small, hg, ALU.mult, ALU.mult, extra_reads=[t_rl])
                            else:
                                S.cp(ACT, tOB, OB[:], t_on[0][s], on[:, 0, s, :])
                            for jn in range(EW // P):
                                pbb = (s * 2 + jn) % 4
                                S.tr(psbt[pbb], psb[:, pbb, :], tOB, OB[:, jn * P:(jn + 1) * P], t_const, CB("ident"))
                                S.cp(POOL if False else DVE, tMO, MO[:, jn, s * P:(s + 1) * P], psbt[pbb], psb[:, pbb, :])
                        r0 = h * EW
                        S.dma(SP, mixT[r0:r0 + EW, qt * TT:(qt + 1) * TT].rearrange("(c p) t -> p c t", p=P), MO[:], tMO, reads=[tMO])
                S.end_phase(LBL[0] if "softattn" == "linear" else "softattn")

        def phase_sb():
            with contextlib.ExitStack() as st:
                kq = [sbt(st, "sk%d" % i, [P, 2, T], BF16) for i in range(2)]
                kqt = [S.t() for _ in range(2)]
                vv = [sbt(st, "sv%d" % i, [P, NCK, P], BF16) for i in range(2)]
                vvt = [S.t() for _ in range(2)]
                E = [sbt(st, "sE%d" % i, [P, TT], F32) for i in range(3)]
                Et = [S.t() for _ in range(3)]
                Lb = [sbt(st, "sL%d" % i, [P, TT], BF16) for i in range(3)]
                Lt = [S.t() for _ in range(3)]
                R = [sbt(st, "sR%d" % i, [P, TT], F32) for i in range(2)]
                Rt = [S.t() for _ in range(2)]
                AG = [sbt(st, "sA%d" % i, [P, TT], F32) for i in range(3)]
                AGt = [S.t() for _ in range(3)]
                Wt = [sbt(st, "sW%d" % i, [P, TT], BF16) for i in range(3)]
                Wtt = [S.t() for _ in range(3)]
                mo = [sbt(st, "smo%d" % i, [P, TT], BF16) for i in range(2)]
                mot = [S.t() for _ in range(2)]
                ps = pst_(st, "ps_s", [P, 8, TT], F32)
                pst = [S.t() for _ in range(8)]

                def load_head(h):
                    i = h % 2
                    rq = 2 * cfg.AW + h * P
                    rk = 2 * cfg.AW + cfg.BW + h * P
                    S.dma(SP, kq[i][:, 0, :], qkT[rk:rk + P, :], kqt[i], writes=[kqt[i]])
                    S.dma(SP, kq[i][:, 1, :], qkT[rq:rq + P, :], kqt[i], writes=[kqt[i]])
                    vc = cfg.AW + h * P
                    S.dma(SP, vv[i][:], vtm[:, vc:vc + P].rearrange("(c p) e -> p c e", p=P), vvt[i], writes=[vvt[i]])
                load_head(0)
                ri = [0]
                for h in range(cfg.BH):
                    if h + 1 < cfg.BH:
                        load_head(h + 1)
                    i = h % 2
                    KQ, tKQ, V, tV = kq[i], kqt[i], vv[i], vvt[i]
                    jobs = []
                    for qt in range(NQT):
                        chi = 4 * qt + 3
                        for c in range(chi, -1, -1):
                            jobs.append((qt, c, c == chi))

                    def stage_A(ix):
                        qt, c, first = jobs[ix]
                        b2, b3 = ix % 2, ix % 3
                        d = c - 4 * qt
                        S.mm(pst[b2], ps[:, b2, :], tKQ, KQ[:, 0, c * P:(c + 1) * P], tKQ, KQ[:, 1, qt * TT:(qt + 1) * TT], True, True)
                        S.act(Et[b3], E[b3][:], pst[b2], ps[:, b2, :], AF.Exp, scale=scale)
                        if d >= 0:
                            S.tt(POOL, Et[b3], E[b3][:], Et[b3], E[b3][:], t_const, CF("m01%d" % d), ALU.mult)
                        S.act(Lt[b3], Lb[b3][:], Et[b3], E[b3][:], AF.Ln, bias=1.0)

                    def stage_B(ix):
                        qt, c, first = jobs[ix]
                        b2, b3 = ix % 2, ix % 3
                        S.mm(pst[2 + b2], ps[:, 2 + b2, :], t_const, CB("tri"), Lt[b3], Lb[b3][:], True, True)
                        if c > 0:
                            S.mm(pst[4 + b2], ps[:, 4 + b2, :], t_const, CB("ones"), Lt[b3], Lb[b3][:], True, True)
                        if first:
                            S.act(AGt[b3], AG[b3][:], pst[2 + b2], ps[:, 2 + b2, :], AF.Exp, scale=-1.0)
                        else:
                            S.tt(DVE, AGt[b3], AG[b3][:], pst[2 + b2], ps[:, 2 + b2, :], Rt[ri[0] % 2], R[ri[0] % 2][:], ALU.add)
                            S.act(AGt[b3], AG[b3][:], AGt[b3], AG[b3][:], AF.Exp, scale=-1.0)
                        if c > 0:
                            if first:
                                S.cp(DVE, Rt[(ri[0] + 1) % 2], R[(ri[0] + 1) % 2][:], pst[4 + b2], ps[:, 4 + b2, :])
                            else:
                                S.tt(DVE, Rt[(ri[0] + 1) % 2], R[(ri[0] + 1) % 2][:], pst[4 + b2], ps[:, 4 + b2, :], Rt[ri[0] % 2], R[ri[0] % 2][:], ALU.add)
                            ri[0] += 1
                        S.tt(POOL, Wtt[b3], Wt[b3][:], Et[b3], E[b3][:], AGt[b3], AG[b3][:], ALU.mult)

                    def stage_C(ix):
                        qt, c, first = jobs[ix]
                        b3 = ix % 3
                        po = 6 + (qt % 2)
                        S.mm(pst[po], ps[:, po, :], tV, V[:, c, :], Wtt[b3], Wt[b3][:], first, c == 0)
                        if c == 0:
                            MO, tMO = mo[qt % 2], mot[qt % 2]
                            S.cp(ACT, tMO, MO[:], pst[po], ps[:, po, :])
                            r0 = cfg.AW + h * P
                            S.dma(SP, mixT[r0:r0 + P, qt * TT:(qt + 1) * TT], MO[:], tMO, reads=[tMO])
                    n = len(jobs)
                    stage_A(0)
                    if n > 1:
                        stage_A(1)
                    stage_B(0)
                    for ix in range(n):
                        if ix + 2 < n:
                            stage_A(ix + 2)
                        if ix + 1 < n:
                            stage_B(ix + 1)
                        stage_C(ix)
                S.end_phase(LBL[0] if "sb" == "linear" else "sb")

        def phase_conv(j):
            HT = T // 2
            with contextlib.ExitStack() as st:
                cw = smallp[:, o_cw:o_cw + KD * 34]
                S.dma(SP, cw[:, 0:KD * 31], convw[j], t_small, writes=[t_small])
                S.dma(SP, cw[:, KD * 31:KD * 32], convb[j], t_small, writes=[t_small])
                S.dma(SP, cw[:, KD * 32:KD * 33], lng[j], t_small, writes=[t_small])
                S.dma(SP, cw[:, KD * 33:KD * 34], lnb[j], t_small, writes=[t_small])
                ag = [sbt(st, "cag%d" % i, [P, 2, T], BF16) for i in range(2)]
                agt = [S.t() for _ in range(2)]
                sig = sbt(st, "csig", [P, T], F32)
                t_sig = S.t()
                hp = sbt(st, "chp", [P, 32 + T], F32)
                t_hp = S.t()
                yy = [sbt(st, "cy%d" % i, [P, T], F32) for i in range(2)]
                yyt = [[S.t(), S.t()] for _ in range(2)]
                yb = sbt(st, "cyb", [P, 2, TT], BF16)
                t_yb = S.t()
                sS = sbt(st, "csS", [P, T], F32)
                sQ = sbt(st, "csQ", [P, T], F32)
                t_sS = [S.t() for _ in range(NQT)]
                t_sQ = [S.t() for _ in range(NQT)]
                ps = pst_(st, "ps_c", [P, 4, TT], F32)
                pst = [S.t() for _ in range(4)]
                r_a = 2 * cfg.CW
                r_g = 2 * cfg.CW + cfg.DCH

                def ld(cc):
                    S.dma(SP, ag[cc % 2][:, 0, :], qkT[r_a + cc * P:r_a + (cc + 1) * P, :], agt[cc % 2], writes=[agt[cc % 2]])
                    S.dma(SP, ag[cc % 2][:, 1, :], qkT[r_g + cc * P:r_g + (cc + 1) * P, :], agt[cc % 2], writes=[agt[cc % 2]])
                ld(0)
                S.ms(DVE, t_hp, hp[:, 0:32], 0.0)
                if CONV_PE:
                    hpb = sbt(st, "chpb", [P, 32 + T], BF16)
                    t_hpb = S.t()
                    S.ms(DVE, t_hpb, hpb[:, 0:32], 0.0)
                    dgm = [sbt(st, "cdg%d" % i, [P, 31, P], BF16) for i in range(2)]
                    dgmt = [S.t() for _ in range(2)]
                for cc in range(KD):
                    if cc + 1 < KD:
                        ld(cc + 1)
                    A, tA = ag[cc % 2], agt[cc % 2]
                    Y, tY = yy[cc % 2], yyt[cc % 2]
                    S.act(t_sig, sig[:], tA, A[:, 1, :], AF.Sigmoid)
                    S.tt(DVE, t_hp, hp[:, 32:32 + T], tA, A[:, 0, :], t_sig, sig[:], ALU.mult)
                    if CONV_PE:
                        Dg, tDg = dgm[cc % 2], dgmt[cc % 2]
                        for tap in range(31):
                            wcol = cw[:, cc * 31 + tap: cc * 31 + tap + 1]
                            S.ts(DVE, tDg, Dg[:, tap, :], t_const, CB("ident"), wcol, None, ALU.mult, extra_reads=[t_small])
                        S.cp(POOL, t_hpb, hpb[:, 32:32 + T], t_hp, hp[:, 32:32 + T])
                        for ti in range(NQT):
                            pb = 2 + (ti % 2)
                            for tap in range(31):
                                S.mm(pst[pb], ps[:, pb, :], tDg, Dg[:, tap, :], t_hpb, hpb[:, 2 + tap + ti * TT: 2 + tap + (ti + 1) * TT], tap == 0, tap == 30)
                            S.act(tY[0], Y[:, ti * TT:(ti + 1) * TT], pst[pb], ps[:, pb, :], AF.Identity, bias=cw[:, KD * 31 + cc:KD * 31 + cc + 1], extra_reads=[t_small])
                    else:
                        for tap in range(31):
                            src = hp[:, 2 + tap: 2 + tap + T]
                            wcol = cw[:, cc * 31 + tap: cc * 31 + tap + 1]
                            if tap == 0:
                                S.ts(DVE, tY[0], Y[:], t_hp, src, wcol, cw[:, KD * 31 + cc:KD * 31 + cc + 1], ALU.mult, ALU.add, extra_reads=[t_small])
                            else:
                                S.stt(tY[0], Y[:], t_hp, src, wcol, tY[0], Y[:], ALU.mult, ALU.add, extra_reads=[t_small])
                    for ti in range(NQT):
                        half = 0
                        ysl = Y[:, ti * TT:(ti + 1) * TT]
                        S.cp(ACT, t_yb, yb[:, 0, :], tY[half], ysl)
                        S.act(t_yb, yb[:, 1, :], tY[half], ysl, AF.Square)
                        S.mm(pst[0], ps[:, 0, :], t_const, CB("onesDCH"), t_yb, yb[:, 0, :], True, True)
                        S.mm(pst[1], ps[:, 1, :], t_const, CB("onesDCH"), t_yb, yb[:, 1, :], True, True)
                        if cc == 0:
                            S.cp(DVE, t_sS[ti], sS[:, ti * TT:(ti + 1) * TT], pst[0], ps[:, 0, :])
                            S.cp(DVE, t_sQ[ti], sQ[:, ti * TT:(ti + 1) * TT], pst[1], ps[:, 1, :])
                        else:
                            S.tt(DVE, t_sS[ti], sS[:, ti * TT:(ti + 1) * TT], pst[0], ps[:, 0, :], t_sS[ti], sS[:, ti * TT:(ti + 1) * TT], ALU.add)
                            S.tt(DVE, t_sQ[ti], sQ[:, ti * TT:(ti + 1) * TT], pst[1], ps[:, 1, :], t_sQ[ti], sQ[:, ti * TT:(ti + 1) * TT], ALU.add)
                    S.dma(SP, ycv[cc * P:(cc + 1) * P, :], Y[:], tY[0], reads=[tY[0]])
                for ti in range(NQT):
                    sl = slice(ti * TT, (ti + 1) * TT)
                    S.tt(DVE, t_sig, sig[:, sl], t_sS[ti], sS[:, sl], t_sS[ti], sS[:, sl], ALU.mult)
                    S.tt(DVE, t_sQ[ti], sQ[:, sl], t_sQ[ti], sQ[:, sl], t_sig, sig[:, sl], ALU.subtract)
                    S.act(t_sQ[ti], sQ[:, sl], t_sQ[ti], sQ[:, sl], AF.Ln, bias=EPS)
                    S.act(t_sQ[ti], sQ[:, sl], t_sQ[ti], sQ[:, sl], AF.Exp, scale=-0.5)
                S.end_phase(LBL[0] if "conv" == "linear" else "conv")
                ob2 = [ag[i][:, 0, :] for i in range(2)]
                ob2t = [S.t() for _ in range(2)]
                t_y2 = [S.t() for _ in range(2)]
                t_st = S.t()

                def ld2(cc):
                    S.dma(SP, yy[cc % 2][:], ycv[cc * P:(cc + 1) * P, :], t_y2[cc % 2], writes=[t_y2[cc % 2]])
                ld2(0)
                for cc in range(KD):
                    if cc + 1 < KD:
                        ld2(cc + 1)
                    Y, tY = yy[cc % 2], t_y2[cc % 2]
                    S.tt(DVE, tY, Y[:], tY, Y[:], t_st, sS[:], ALU.subtract)
                    S.tt(POOL, tY, Y[:], tY, Y[:], t_st, sQ[:], ALU.mult)
                    S.act(ob2t[cc % 2], ob2[cc % 2], tY, Y[:], AF.Silu, bias=cw[:, KD * 33 + cc:KD * 33 + cc + 1],
                          scale=cw[:, KD * 32 + cc:KD * 32 + cc + 1], extra_reads=[t_small])
                    r0 = cfg.CW + cc * P
                    S.dma(SP, mixT[r0:r0 + P, :], ob2[cc % 2], ob2t[cc % 2], reads=[ob2t[cc % 2]])
                S.end_phase(LBL[0] if "conv" == "linear" else "conv")

        def down(x_dram, K, W, N, x_src, x_dst, gcol):
            kct = int(_os.environ.get("KDBG_KCT", "16"))
            if K // P > kct:
                phase_down(x_dram, K, W, N, x_src, x_dst, gcol)
            else:
                phase_linear(x_dram, K, [W], N, "resid", x_src=x_src, x_dst=x_dst, gcol=gcol)

        for l in layers:
            j = l // 2
            phase_ada(l)
            first = (l == layers[0])
            lastl = (l == layers[-1])
            for b in range(NB):
                x0 = xT_in[b] if first else xres[b]
                fin = yT_out[b] if lastl else xres[b]
                phase_norm(b, x0, 0)
                qg = smallp[:, o_misc + 10:o_misc + 11]
                kg = smallp[:, o_misc + 11:o_misc + 12]
                if l % 2 == 0:
                    S.dma(SP, qg, aqg[j], t_small, writes=[t_small])
                    S.dma(SP, kg, akg[j], t_small, writes=[t_small])
                    W = ev_w_in[j]
                    AW, BW = cfg.AW, cfg.BW
                    phase_linear(hT, D, [W[:, 0:AW]], AW, "qknorm", gcol=qg, dst=qkT, dst_row=0)
                    phase_linear(hT, D, [W[:, AW:2 * AW]], AW, "qknorm", gcol=kg, dst=qkT, dst_row=AW)
                    phase_linear(hT, D, [W[:, 2 * AW:3 * AW]], AW, "tm", dst_col=0)
                    phase_linear(hT, D, [W[:, 3 * AW:3 * AW + 2 * BW]], 2 * BW, "plain", dst=qkT, dst_row=2 * AW)
                    phase_linear(hT, D, [W[:, 3 * AW + 2 * BW:3 * AW + 3 * BW]], BW, "tm", dst_col=AW)
                    phase_softattn("diff", l)
                    phase_sb()
                    wout = ev_w_out[j]
                else:
                    S.dma(SP, qg, cqg[j], t_small, writes=[t_small])
                    S.dma(SP, kg, ckg[j], t_small, writes=[t_small])
                    W = od_w_in[j]
                    CW = cfg.CW
                    phase_linear(hT, D, [W[:, 0:CW]], CW, "qknorm", gcol=qg, dst=qkT, dst_row=0)
                    phase_linear(hT, D, [W[:, CW:2 * CW]], CW, "qknorm", gcol=kg, dst=qkT, dst_row=CW)
                    phase_linear(hT, D, [W[:, 2 * CW:3 * CW]], CW, "tm", dst_col=0)
                    phase_linear(hT, D, [W[:, 3 * CW:3 * CW + 2 * cfg.DCH]], 2 * cfg.DCH, "plain", dst=qkT, dst_row=2 * CW)
                    phase_softattn("dil", l)
                    phase_conv(j)
                    wout = od_w_out[j]
                phase_linear(mixT, D, [wout], D, "resid", x_src=x0, x_dst=xb, gcol=ada_col(b, 2))
                if l % 2 == 0:
                    phase_norm(b, xb, 1)
                    phase_linear(hT, D, [ffn_wg[j], ffn_wu[j]], DFF, "glu", dst=AT, dst_row=0)
                    down(AT, DFF, ffn_wd[j], D, xb, fin, ada_col(b, 5))
                else:
                    phase_norm(b, xb, 1, router_l=j)
                    cur, other = xb, xc
                    for ex in range(NE):
                        phase_linear(hT, D, [moe_wg[j, ex], moe_wu[j, ex]], DFF, "glu", dst=AT, dst_row=0, gate_e=ex)
                        dst = fin if ex == NE - 1 else other
                        down(AT, DFF, moe_wd[j, ex], D, cur, dst, ada_col(b, 5))
                        cur, other = dst, cur
    build_program.stats = S.stats
    return nc, (ab, af, ab2)


def _colmajor(v, kc):
    v = np.asarray(v, np.float32)
    lead = v.shape[:-1]
    return np.ascontiguousarray(np.swapaxes(v.reshape(*lead, kc, P), -1, -2))


def make_in_maps(cfg, inp, consts):
    ab, af, ab2 = consts
    D, T, NB, KC, NE = cfg.D, cfg.T, cfg.NB, cfg.KC, cfg.NE
    KD = cfg.DCH // P
    f = lambda a: np.ascontiguousarray(np.asarray(a, np.float32))
    shared = {
        "ada_w": f(inp["ada_w"]),
        "ada_bT": _colmajor(inp["ada_b"], 6 * KC),
        "nmgT": _colmajor(inp["norm_mix_g"], KC),
        "nfgT": _colmajor(inp["norm_ffn_g"], KC),
        "ev_w_in": f(inp["ev_w_in"]), "ev_w_out": f(inp["ev_w_out"]),
        "aqg": f(np.asarray(inp["a_q_norm_g"])[:, :, None]), "akg": f(np.asarray(inp["a_k_norm_g"])[:, :, None]),
        "alam": f(np.transpose(np.asarray(inp["a_lambda"]), (0, 2, 1))),
        "ahg": f(np.broadcast_to(np.asarray(inp["a_head_norm_g"])[:, None, :], (cfg.NEV, P, 256))),
        "ffn_wg": f(inp["ffn_w_gate"]), "ffn_wu": f(inp["ffn_w_up"]), "ffn_wd": f(inp["ffn_w_down"]),
        "od_w_in": f(inp["od_w_in"]), "od_w_out": f(inp["od_w_out"]),
        "cqg": f(np.asarray(inp["c_q_norm_g"])[:, :, None]), "ckg": f(np.asarray(inp["c_k_norm_g"])[:, :, None]),
        "convw": f(np.transpose(np.asarray(inp["d_conv_w"]).reshape(cfg.NOD, 31, KD, P), (0, 3, 2, 1)).reshape(cfg.NOD, P, KD * 31)),
        "convb": _colmajor(inp["d_conv_b"], KD), "lng": _colmajor(inp["d_ln_g"], KD), "lnb": _colmajor(inp["d_ln_b"], KD),
        "routT": f(np.transpose(np.asarray(inp["moe_router"]).reshape(cfg.NOD, KC, P, NE), (0, 2, 1, 3)).reshape(cfg.NOD, P, KC * NE)),
        "moe_wg": f(inp["moe_w_gate"]), "moe_wu": f(inp["moe_w_up"]), "moe_wd": f(inp["moe_w_down"]),
        "cb": ab, "cf": af, "cb2": ab2,
    }
    x = np.asarray(inp["x"], np.float32)
    c = np.asarray(inp["c"], np.float32)
    maps = []
    for core in range(cfg.ncores):
        bs = range(core * NB, (core + 1) * NB)
        m = dict(shared)
        m["xT"] = np.ascontiguousarray(np.stack([x[b].T for b in bs]))
        cc = np.stack([c[b].reshape(KC, P).T for b in bs], axis=1)
        m["cT"] = np.ascontiguousarray(cc.reshape(P, NB * KC))
        maps.append(m)
    return maps


def run(cfg, inp, layers=None):
    nc, consts = build_program(cfg, layers)
    maps = make_in_maps(cfg, inp, consts)
    res = run_bass_kernel_spmd(nc, maps, core_ids=list(range(cfg.ncores)))
    outs = []
    for core in range(cfg.ncores):
        yT = res.results[core]["yT"]
        for b in range(cfg.NB):
            outs.append(np.ascontiguousarray(yT[b].T))
    return np.stack(outs).astype(np.float32)


NCORES = 8


def kernel(**inputs):
    cfg = Cfg(D=2048, T=4096, NB=8 // NCORES, L=4, NE=8, ncores=NCORES)
    return run(cfg, inputs)
```

```python
import contextlib
import math
import numpy as np
import ml_dtypes
import concourse.bass as bass
import concourse.mybir as mybir
from concourse.bass_utils import run_bass_kernel_spmd

F32 = mybir.dt.float32
BF16 = mybir.dt.bfloat16
AF = mybir.ActivationFunctionType
ALU = mybir.AluOpType
PE, ACT, DVE, POOL, SP = "pe", "act", "dve", "pool", "sp"
P = 128
TT = 512
EPS = 1e-6
NEG = -30000.0
import os as _os
EMBED_WAIT = _os.environ.get("KDBG_EMBED", "1") == "1"
CONV_PE = _os.environ.get("KDBG_CONVPE", "1") == "1"


class Cfg:
    def __init__(self, D=2048, T=4096, NB=1, L=4, NE=8, ncores=8):
        self.D, self.T, self.NB, self.L, self.NE, self.ncores = D, T, NB, L, NE, ncores
        self.KC = D // P
        self.AH = D // 512
        self.AW = self.AH * 256
        self.BH = D // 256
        self.BW = self.BH * 128
        self.EVEN_IN = 3 * self.AW + 3 * self.BW
        self.CH = D // 256
        self.CW = self.CH * 128
        self.DCH = D // 2
        self.ODD_IN = 3 * self.CW + 2 * self.DCH
        self.DFF = ((8 * D // 3 + 255) // 256) * 256
        self.KF = self.DFF // P
        self.NEV = (L + 1) // 2
        self.NOD = L // 2
        self.NQT = T // TT
        self.NCK = T // P


class Tl:
    __slots__ = ("w", "r", "dsem")

    def __init__(self):
        self.w = None
        self.r = {}
        self.dsem = None


class Sched:
    def __init__(self, nc, st):
        self.nc = nc
        self.st = st
        self.ops = {e: [] for e in (PE, ACT, DVE, POOL, SP)}
        self.cnt = {e: 0 for e in self.ops}
        self.seen = {e: {} for e in self.ops}
        self.dcount = {}
        self.nd = 0
        self.sems = {}

    def t(self):
        return Tl()

    def _deps(self, eng, reads, writes, pe_accum):
        need = {}
        for t in reads:
            if t.w is not None and need.get(t.w[0], 0) < t.w[1]:
                need[t.w[0]] = t.w[1]
        for t in writes:
            if t.w is not None and not (pe_accum and t.w[0] == PE and eng == PE):
                if need.get(t.w[0], 0) < t.w[1]:
                    need[t.w[0]] = t.w[1]
            for k, v in t.r.items():
                if need.get(k, 0) < v:
                    need[k] = v
        waits = []
        seen = self.seen[eng]
        for k, v in need.items():
            if k == PE and eng == PE:
                continue
            if seen.get(k, 0) >= v:
                continue
            seen[k] = v
            waits.append((k, v))
        return waits

    def op(self, eng, fn, reads=(), writes=(), pe_accum=False):
        waits = self._deps(eng, reads, writes, pe_accum)
        self.cnt[eng] += 1
        c = self.cnt[eng]
        self.ops[eng].append((waits, fn, (eng, 1)))
        for t in reads:
            t.r[eng] = c
        for t in writes:
            t.w = (eng, c)
            t.r = {}

    def dma(self, q, out_ap, in_ap, own, reads=(), writes=()):
        if own.dsem is None:
            own.dsem = ("d", self.nd)
            self.nd += 1
            self.dcount.setdefault(own.dsem, 0)
        waits = self._deps(q, reads, writes, False)
        self.dcount[own.dsem] += 16
        v = self.dcount[own.dsem]
        self.ops[q].append((waits, lambda e: e.dma_start(out=out_ap, in_=in_ap), (own.dsem, 16)))
        for t in reads:
            t.r[own.dsem] = v
        for t in writes:
            t.w = (own.dsem, v)
            t.r = {}

    def _sem(self, k):
        if k not in self.sems:
            self.sems[k] = self.st.enter_context(self.nc.semaphore("s_%s" % (k if isinstance(k, str) else "d%d" % k[1])))
        return self.sems[k]

    def end_phase(self, label="ph"):
        import os
        self.nphase = getattr(self, "nphase", 0) + 1
        lim = int(os.environ.get("KDBG_STOP", "0"))
        if lim and self.nphase > lim:
            for e in self.ops:
                self.ops[e] = []
            self.nd = 0
            return
        for e in self.ops:
            waits = []
            for k in (PE, ACT, DVE, POOL):
                if k != e and self.cnt[k] > self.seen[e].get(k, 0):
                    waits.append((k, self.cnt[k]))
                    self.seen[e][k] = self.cnt[k]
            for k, v in self.dcount.items():
                if v > self.seen[e].get(k, 0):
                    waits.append((k, v))
                    self.seen[e][k] = v
            if waits:
                self.ops[e].append((waits, None, None))
        nc = self.nc
        with nc.named_scope("p%03d_%s" % (self.nphase, label)), nc.Block() as block:
            def run(name):
                def body(eobj):
                    for waits, fn, inc in self.ops[name]:
                        if fn is None or not EMBED_WAIT or not waits:
                            for k, v in waits:
                                eobj.wait_ge(self._sem(k), v)
                            if fn is not None:
                                fn(eobj).then_inc(self._sem(inc[0]), inc[1])
                        else:
                            for k, v in waits[1:]:
                                eobj.wait_ge(self._sem(k), v)
                            ins = fn(eobj)
                            ins._wait_ge(self._sem(waits[0][0]), waits[0][1])
                            ins.then_inc(self._sem(inc[0]), inc[1])
                return body
            block.tensor(run(PE))
            block.scalar(run(ACT))
            block.vector(run(DVE))
            block.gpsimd(run(POOL))
            block.sync(run(SP))
        st = getattr(self, 'stats', {})
        for e in self.ops:
            st[e] = st.get(e, 0) + sum(1 for w, f, i in self.ops[e] if f is not None)
            st['w_' + e] = st.get('w_' + e, 0) + sum(len(w) for w, f, i in self.ops[e])
        self.stats = st
        for e in self.ops:
            self.ops[e] = []
        self.nd = 0

    def mm(self, out_t, out_ap, l_t, l_ap, r_t, r_ap, start, stop):
        self.op(PE, lambda e: e.matmul(out_ap, lhsT=l_ap, rhs=r_ap, start=start, stop=stop), [l_t, r_t], [out_t], pe_accum=True)

    def tr(self, out_t, out_ap, i_t, i_ap, id_t, id_ap):
        self.op(PE, lambda e: e.transpose(out=out_ap, in_=i_ap, identity=id_ap), [i_t, id_t], [out_t])

    def act(self, out_t, out_ap, in_t, in_ap, func, bias=None, scale=None, extra_reads=(), eng=ACT):
        kw = {}
        if bias is not None:
            kw["bias"] = bias
        if scale is not None:
            kw["scale"] = scale
        self.op(eng, lambda e: e.activation(out=out_ap, in_=in_ap, func=func, **kw), [in_t] + list(extra_reads), [out_t])

    def tt(self, eng, out_t, out_ap, a_t, a_ap, b_t, b_ap, op):
        self.op(eng, lambda e: e.tensor_tensor(out=out_ap, in0=a_ap, in1=b_ap, op=op), [a_t, b_t], [out_t])

    def ts(self, eng, out_t, out_ap, a_t, a_ap, s1, s2, op0, op1=None, extra_reads=()):
        if op1 is None:
            self.op(eng, lambda e: e.tensor_scalar(out=out_ap, in0=a_ap, scalar1=s1, scalar2=None, op0=op0), [a_t] + list(extra_reads), [out_t])
        else:
            self.op(eng, lambda e: e.tensor_scalar(out=out_ap, in0=a_ap, scalar1=s1, scalar2=s2, op0=op0, op1=op1), [a_t] + list(extra_reads), [out_t])

    def stt(self, out_t, out_ap, a_t, a_ap, scalar, b_t, b_ap, op0, op1, extra_reads=()):
        self.op(DVE, lambda e: e.scalar_tensor_tensor(out=out_ap, in0=a_ap, scalar=scalar, in1=b_ap, op0=op0, op1=op1), [a_t, b_t] + list(extra_reads), [out_t])

    def cp(self, eng, out_t, out_ap, in_t, in_ap):
        if eng == ACT:
            self.op(ACT, lambda e: e.activation(out=out_ap, in_=in_ap, func=AF.Copy), [in_t], [out_t])
        else:
            self.op(eng, lambda e: e.tensor_copy(out=out_ap, in_=in_ap), [in_t], [out_t])

    def ms(self, eng, out_t, out_ap, val):
        self.op(eng, lambda e: e.memset(out_ap, val), [], [out_t])


def alibi(n):
    return [2.0 ** (-8.0 * (h + 1) / n) for h in range(n)]


def make_consts(cfg):
    scale = 128 ** -0.5
    j = np.arange(P)[:, None].astype(np.float64)
    i = np.arange(TT)[None, :].astype(np.float64)
    cb = {}
    cb["ident"] = np.eye(P)
    cb["ones"] = np.ones((P, P))
    cb["onesD"] = np.full((P, P), 1.0 / cfg.D)
    cb["ones128"] = np.full((P, P), 1.0 / 128)
    cb["onesDCH"] = np.full((P, P), 1.0 / cfg.DCH)
    s = np.arange(P)[None, :]
    cb["tri"] = (np.arange(P)[:, None] >= s).astype(np.float64)
    for d in range(4):
        cb["mneg%d" % d] = np.where(i >= 128 * d + j, 0.0, NEG)

    def count(dl):
        return ((dl >= 0) & (dl <= 128)).astype(np.float64) + ((dl >= 0) & (dl <= 512) & (dl % 4 == 0)) + \
            ((dl >= 0) & (dl <= 2048) & (dl % 16 == 0))
    for u in range(20):
        d0 = 128 * (u - 3)
        cb["cnt%d" % u] = count((d0 + i - j).astype(np.int64))
    slc = alibi(cfg.CH)
    for h in range(cfg.CH):
        cb["shift%d" % h] = np.broadcast_to(-(slc[h] / scale) * (i - 256.0), (P, TT))
    cf = {}
    cf["identf"] = np.eye(P)
    cf["onesf"] = np.ones((P, P))
    for d in range(4):
        cf["m01%d" % d] = (i > 128 * d + j).astype(np.float64)
    sla = alibi(cfg.AH)
    dc = np.arange(-31, 4)[None, :]
    for h in range(cfg.AH):
        cf["biasA%d" % h] = sla[h] * (128.0 * dc + j - 256.0)
    dc2 = np.arange(-16, 4)[None, :]
    for h in range(cfg.CH):
        cf["biasC%d" % h] = slc[h] * (128.0 * dc2 + j - 256.0)

    def pack(dct, dt):
        offs, cols, o = {}, [], 0
        for k, v in dct.items():
            v = np.asarray(v)
            offs[k] = (o, v.shape[1])
            o += v.shape[1]
            cols.append(v)
        return offs, np.ascontiguousarray(np.concatenate(cols, axis=1).astype(np.float32)).astype(dt)
    cb2 = {k: cb.pop(k) for k in list(cb) if k.startswith("cnt") or k.startswith("shift")}
    ob, ab = pack(cb, ml_dtypes.bfloat16)
    ob2, ab2 = pack(cb2, ml_dtypes.bfloat16)
    of, af = pack(cf, np.float32)
    return ob, ab, of, af, ob2, ab2


def build_program(cfg, layers=None):
    if layers is None:
        layers = list(range(cfg.L))
    D, T, NB, KC, KF, NE, DFF = cfg.D, cfg.T, cfg.NB, cfg.KC, cfg.KF, cfg.NE, cfg.DFF
    NQT, NCK = cfg.NQT, cfg.NCK
    ob, ab, of, af, ob2, ab2 = make_consts(cfg)
    nc = bass.Bass("TRN2", target_bir_lowering=False)

    def din(name, shape, dt=F32):
        return nc.dram_tensor(name, list(shape), dt, kind="ExternalInput").ap()

    def dscr(name, shape, dt):
        return nc.dram_tensor(name, list(shape), dt, kind="Internal").ap()

    L, NEV, NOD = cfg.L, cfg.NEV, cfg.NOD
    xT_in = din("xT", [NB, D, T])
    cT_in = din("cT", [P, NB * KC])
    ada_w = din("ada_w", [L, D, 6 * D])
    ada_bT = din("ada_bT", [L, P, 6 * KC])
    nmgT = din("nmgT", [L, P, KC])
    nfgT = din("nfgT", [L, P, KC])
    ev_w_in = din("ev_w_in", [NEV, D, cfg.EVEN_IN])
    ev_w_out = din("ev_w_out", [NEV, D, D])
    aqg = din("aqg", [NEV, P, 1])
    akg = din("akg", [NEV, P, 1])
    alam = din("alam", [NEV, P, 4])
    ahg = din("ahg", [NEV, P, 256])
    ffn_wg = din("ffn_wg", [NEV, D, DFF])
    ffn_wu = din("ffn_wu", [NEV, D, DFF])
    ffn_wd = din("ffn_wd", [NEV, DFF, D])
    od_w_in = din("od_w_in", [NOD, D, cfg.ODD_IN])
    od_w_out = din("od_w_out", [NOD, D, D])
    cqg = din("cqg", [NOD, P, 1])
    ckg = din("ckg", [NOD, P, 1])
    KD = cfg.DCH // P
    convw = din("convw", [NOD, P, KD * 31])
    convb = din("convb", [NOD, P, KD])
    lng = din("lng", [NOD, P, KD])
    lnb = din("lnb", [NOD, P, KD])
    routT = din("routT", [NOD, P, KC * NE])
    moe_wg = din("moe_wg", [NOD, NE, D, DFF])
    moe_wu = din("moe_wu", [NOD, NE, D, DFF])
    moe_wd = din("moe_wd", [NOD, NE, DFF, D])
    cb_in = din("cb", list(ab.shape), BF16)
    cf_in = din("cf", list(af.shape), F32)
    cb2_in = din("cb2", list(ab2.shape), BF16)
    yT_out = nc.dram_tensor("yT", [NB, D, T], F32, kind="ExternalOutput").ap()

    xres = dscr("xres", [NB, D, T], F32)
    xb = dscr("xb", [D, T], F32)
    xc = dscr("xc", [D, T], F32)
    hT = dscr("hT", [D, T], BF16)
    QKR = max(2 * cfg.AW + 2 * cfg.BW, 2 * cfg.CW + 2 * cfg.DCH)
    qkT = dscr("qkT", [QKR, T], BF16)
    VW = max(cfg.AW + cfg.BW, cfg.CW)
    vtm = dscr("vtm", [T, VW], BF16)
    mixT = dscr("mixT", [D, T], BF16)
    AT = dscr("AT", [DFF, T], BF16)
    gatesB = dscr("gatesB", [NE, P, T], F32)
    ycv = dscr("ycv", [cfg.DCH, T], F32)

    scale = 128 ** -0.5

    with contextlib.ExitStack() as top:
        S = Sched(nc, top)

        uid = [0]
        LBL = ["lin"]

        def sbt(st, name, shape, dt):
            uid[0] += 1
            return st.enter_context(nc.sbuf_tensor("%s_u%d" % (name, uid[0]), list(shape), dt))

        def pst_(st, name, shape, dt):
            uid[0] += 1
            return st.enter_context(nc.psum_tensor("%s_u%d" % (name, uid[0]), list(shape), dt))

        cbs = sbt(top, "cbs", ab.shape, BF16)
        cfs = sbt(top, "cfs", af.shape, F32)
        condT = sbt(top, "condT", [P, NB * KC], F32)
        adaT = sbt(top, "adaT", [P, NB * 6 * KC], F32)
        colA = sbt(top, "colA", [P, NB * 2 * KC], F32)
        smallp = sbt(top, "smallp", [P, 2 * KC + 16 + 256 + KD * 34 + KC * NE], F32)
        t_const = S.t()
        t_cond = S.t()
        t_ada = S.t()
        t_colA = S.t()
        t_small = S.t()

        def CB(name, rows=slice(0, P)):
            o, w = ob[name]
            return cbs[rows, o:o + w]

        def CF(name, rows=slice(0, P)):
            o, w = of[name]
            return cfs[rows, o:o + w]

        o_ng = 0
        o_misc = 2 * KC
        o_hg = o_misc + 16
        o_cw = o_hg + 256
        o_rt = o_cw + KD * 34

        S.dma(SP, cbs[:], cb_in, t_const, writes=[t_const])
        S.dma(SP, cfs[:], cf_in, t_const, writes=[t_const])
        S.dma(SP, condT[:], cT_in, t_cond, writes=[t_cond])
        S.act(t_cond, condT[:], t_cond, condT[:], AF.Silu)
        S.end_phase(LBL[0] if "init" == "linear" else "init")

        def phase_ada(l):
            with contextlib.ExitStack() as st:
                NBLK = 6 * D // TT
                wt = [sbt(st, "adaw%d" % i, [P, KC, TT], F32) for i in range(2)]
                wtt = [S.t() for _ in range(2)]
                row = sbt(st, "adarow", [1, NB * 6 * D], F32)
                t_row = S.t()
                bT = sbt(st, "adab", [P, 6 * KC], F32)
                t_b = S.t()
                ng = sbt(st, "ng", [P, 2 * KC], F32)
                t_ng = S.t()
                ps = pst_(st, "ps_ada", [P, 4, TT], F32)
                pst = [S.t() for _ in range(4)]
                S.dma(SP, bT[:], ada_bT[l], t_b, writes=[t_b])
                S.dma(SP, ng[:, 0:KC], nmgT[l], t_ng, writes=[t_ng])
                S.dma(SP, ng[:, KC:2 * KC], nfgT[l], t_ng, writes=[t_ng])
                wsrc = ada_w[l].rearrange("(kc p) n -> p kc n", p=P)

                def ld(i):
                    S.dma(SP if i % 2 == 0 else ACT, wt[i % 2][:], wsrc[:, :, i * TT:(i + 1) * TT], wtt[i % 2], writes=[wtt[i % 2]])
                ld(0)
                k = 0
                for i in range(NBLK):
                    if i + 1 < NBLK:
                        ld(i + 1)
                    for b in range(NB):
                        pb = k % 3
                        k += 1
                        for kc in range(KC):
                            S.mm(pst[pb], ps[0:1, pb, :], t_cond, condT[:, b * KC + kc:b * KC + kc + 1],
                                 wtt[i % 2], wt[i % 2][:, kc, :], kc == 0, kc == KC - 1)
                        S.cp(ACT, t_row, row[0:1, b * 6 * D + i * TT: b * 6 * D + (i + 1) * TT], pst[pb], ps[0:1, pb, :])
                for b in range(NB):
                    for jn in range(6 * KC):
                        S.mm(pst[3], ps[:, 3, b * 6 * KC + jn: b * 6 * KC + jn + 1], t_row,
                             row[0:1, b * 6 * D + jn * P: b * 6 * D + (jn + 1) * P], t_const, CF("onesf", slice(0, 1))[:, 0:1], True, True)
                for b in range(NB):
                    S.tt(DVE, t_ada, adaT[:, b * 6 * KC:(b + 1) * 6 * KC], pst[3], ps[:, 3, b * 6 * KC:(b + 1) * 6 * KC], t_b, bT[:], ALU.add)
                    for which, (osc, og) in enumerate(((KC, 0), (4 * KC, KC))):
                        dst = colA[:, (b * 2 + which) * KC:(b * 2 + which + 1) * KC]
                        S.ts(DVE, t_colA, dst, t_ada, adaT[:, b * 6 * KC + osc: b * 6 * KC + osc + KC], 1.0, None, ALU.add)
                        S.tt(DVE, t_colA, dst, t_colA, dst, t_ng, ng[:, og:og + KC], ALU.mult)
                S.end_phase(LBL[0] if "ada" == "linear" else "ada")

        def ada_col(b, which):
            return adaT[:, b * 6 * KC + which * KC: b * 6 * KC + (which + 1) * KC]

        def phase_norm(b, x_src, sub, router_l=None):
            Acol = colA[:, (b * 2 + sub) * KC:(b * 2 + sub + 1) * KC]
            Bcol = ada_col(b, 0 if sub == 0 else 3)
            TN = 256 if router_l is not None else 512
            with contextlib.ExitStack() as st:
                xt = [sbt(st, "nx%d" % i, [P, KC, TN], F32) for i in range(2)]
                xtt = [S.t() for _ in range(2)]
                sq = sbt(st, "nsq", [P, KC, TN], BF16)
                t_sq = S.t()
                hb = [sbt(st, "nhb%d" % i, [P, KC, TN], BF16) for i in range(2)]
                hbt = [S.t() for _ in range(2)]
                rstd = sbt(st, "nrstd", [P, TN], F32)
                t_rstd = S.t()
                tmp = [sbt(st, "ntmp%d" % i, [P, TN], F32) for i in range(2)]
                tmpt = [S.t() for _ in range(2)]
                ps = pst_(st, "ps_n", [P, 4, TT], F32)
                pst = [S.t() for _ in range(4)]
                if router_l is not None:
                    rwf = sbt(st, "nrwf", [P, KC * NE], F32)
                    t_rwf = S.t()
                    S.dma(SP, rwf[:], routT[router_l], t_rwf, writes=[t_rwf])
                    rw = sbt(st, "nrw", [P, KC * NE], BF16)
                    t_rw = S.t()
                    S.cp(DVE, t_rw, rw[:], t_rwf, rwf[:])
                    lg = sbt(st, "nlg", [P, 8 * NE], F32)
                    t_lg = S.t()
                    dg = [sbt(st, "ndg%d" % i, [P, P], BF16) for i in range(2)]
                    dgt = [S.t() for _ in range(2)]
                    gb = [sbt(st, "ngb%d" % i, [P, NE, TN], F32) for i in range(2)]
                    gbt = [S.t() for _ in range(2)]
                xsrc = x_src.rearrange("(kc p) t -> p kc t", p=P)
                hdst = hT.rearrange("(kc p) t -> p kc t", p=P)

                def ld(i):
                    S.dma(SP, xt[i % 2][:], xsrc[:, :, i * TN:(i + 1) * TN], xtt[i % 2], writes=[xtt[i % 2]])
                ld(0)
                for i in range(T // TN):
                    if i + 1 < T // TN:
                        ld(i + 1)
                    X, tX = xt[i % 2], xtt[i % 2]
                    H, tH = hb[i % 2], hbt[i % 2]
                    S.act(t_sq, sq[:], tX, X[:], AF.Square)
                    for kc in range(KC):
                        S.mm(pst[0], ps[:, 0, 0:TN], t_const, CB("onesD"), t_sq, sq[:, kc, :], kc == 0, kc == KC - 1)
                    S.act(t_rstd, rstd[:], pst[0], ps[:, 0, 0:TN], AF.Ln, bias=EPS)
                    S.act(t_rstd, rstd[:], t_rstd, rstd[:], AF.Exp, scale=-0.5)
                    for kc in range(KC):
                        tm, ttm = tmp[kc % 2], tmpt[kc % 2]
                        S.stt(ttm, tm[:], tX, X[:, kc, :], Acol[:, kc:kc + 1], t_rstd, rstd[:], ALU.mult, ALU.mult, extra_reads=[t_colA])
                        S.act(tH, H[:, kc, :], ttm, tm[:], AF.Identity, bias=Bcol[:, kc:kc + 1], extra_reads=[t_ada])
                    S.dma(SP, hdst[:, :, i * TN:(i + 1) * TN], H[:], tH, reads=[tH])
                    if router_l is not None:
                        G, tG = gb[i % 2], gbt[i % 2]
                        for s in range(TN // P):
                            pl = ps[:, 1, s * NE:(s + 1) * NE]
                            for kc in range(KC):
                                S.mm(pst[1], pl, tH, H[:, kc, s * P:(s + 1) * P], t_rw, rw[:, kc * NE:(kc + 1) * NE], kc == 0, kc == KC - 1)
                            lgs = lg[:, 0:NE]
                            m1 = lg[:, NE:NE + 1]
                            eq = lg[:, 2 * NE:3 * NE]
                            l2 = lg[:, 3 * NE:4 * NE]
                            m2 = lg[:, 4 * NE:4 * NE + 1]
                            ex = lg[:, 5 * NE:6 * NE]
                            den = lg[:, 6 * NE:6 * NE + 1]
                            gt = lg[:, 7 * NE:8 * NE]
                            S.cp(DVE, t_lg, lgs, pst[1], pl)
                            S.op(DVE, lambda e, a=m1, b_=lgs: e.reduce_max(out=a, in_=b_, axis=mybir.AxisListType.X), [t_lg], [t_lg])
                            S.ts(DVE, t_lg, eq, t_lg, lgs, m1, None, ALU.is_ge)
                            S.stt(t_lg, l2, t_lg, eq, -10000.0, t_lg, lgs, ALU.mult, ALU.add)
                            S.op(DVE, lambda e, a=m2, b_=l2: e.reduce_max(out=a, in_=b_, axis=mybir.AxisListType.X), [t_lg], [t_lg])
                            S.ts(DVE, t_lg, eq, t_lg, lgs, m2, None, ALU.is_ge)
                            S.ts(DVE, t_lg, ex, t_lg, lgs, m1, None, ALU.subtract)
                            S.act(t_lg, ex, t_lg, ex, AF.Exp)
                            S.tt(DVE, t_lg, ex, t_lg, ex, t_lg, eq, ALU.mult)
                            S.op(DVE, lambda e, a=den, b_=ex: e.reduce_sum(out=a, in_=b_, axis=mybir.AxisListType.X), [t_lg], [t_lg])
                            S.op(DVE, lambda e, a=den: e.reciprocal(out=a, in_=a), [t_lg], [t_lg])
                            S.ts(DVE, t_lg, gt, t_lg, ex, den, None, ALU.mult)
                            for ex_ in range(NE):
                                dd, tdd = dg[ex_ % 2], dgt[ex_ % 2]
                                S.ts(DVE, tdd, dd[:], t_const, CB("ident"), gt[:, ex_:ex_ + 1], None, ALU.mult, extra_reads=[t_lg])
                                pb = 2 + (ex_ % 2)
                                S.mm(pst[pb], ps[:, pb, 0:P], t_const, CB("ones"), tdd, dd[:], True, True)
                                S.cp(ACT, tG, G[:, ex_, s * P:(s + 1) * P], pst[pb], ps[:, pb, 0:P])
                        S.dma(SP, gatesB.rearrange("e p t -> p e t")[:, :, i * TN:(i + 1) * TN], G[:], tG, reads=[tG])
                S.end_phase(LBL[0] if "norm" == "linear" else "norm")

        def phase_linear(x_dram, K, Ws, N, mode, **kw):
            KCk = K // P
            LBL[0] = "lin_%s_K%d_N%d" % (mode, K, N)
            import os
            kct = int(os.environ.get("KDBG_KCT", "16"))
            nbw = TT if KCk <= kct else 256
            TX = TT if KCk <= kct else 256
            NQX = T // TX
            if N % nbw:
                nbw = 256
            assert N % nbw == 0
            NBK = N // nbw
            NJ = nbw // P
            nW = len(Ws)
            with contextlib.ExitStack() as st:
                wb = [[sbt(st, "lw%d_%d" % (w, i), [P, KCk, nbw], BF16) for i in range(2)] for w in range(nW)]
                wbt = [[S.t() for _ in range(2)] for _ in range(nW)]
                xt = [sbt(st, "lx%d" % i, [P, KCk, TX], BF16) for i in range(3)]
                xtt = [S.t() for _ in range(3)]
                ps = pst_(st, "ps_l", [P, 8, TT], F32)
                pst = [S.t() for _ in range(8)]
                xsrc = x_dram.rearrange("(kc p) t -> p kc t", p=P)
                wsrc = [W.rearrange("(kc p) n -> p kc n", p=P) for W in Ws]
                if mode in ("plain", "qknorm", "glu"):
                    og = [sbt(st, "lo%d" % i, [P, NJ, TX], BF16) for i in range(2)]
                    ogt = [S.t() for _ in range(2)]
                if mode == "qknorm":
                    sq = [sbt(st, "lsq%d" % i, [P, TX], BF16) for i in range(2)]
                    sqt = [S.t() for _ in range(2)]
                    rs = [sbt(st, "lrs%d" % i, [P, TX], F32) for i in range(2)]
                    rst = [S.t() for _ in range(2)]
                if mode == "glu":
                    sg = [sbt(st, "lsg%d" % i, [P, TX], F32) for i in range(2)]
                    sgt = [S.t() for _ in range(2)]
                    if kw.get("gate_e") is not None:
                        gbx = [sbt(st, "lgb%d" % i, [P, TX], F32) for i in range(3)]
                        gbxt = [S.t() for _ in range(3)]
                if mode == "tm":
                    ot = [sbt(st, "lot%d" % i, [P, TX // P, nbw], BF16) for i in range(2)]
                    ott = [S.t() for _ in range(2)]
                if mode == "resid":
                    xi = [sbt(st, "lxi%d" % i, [P, NJ, TX], F32) for i in range(3)]
                    xit = [S.t() for _ in range(3)]
                    xo = [sbt(st, "lxo%d" % i, [P, NJ, TX], F32) for i in range(2)]
                    xot = [S.t() for _ in range(2)]
                    rsrc = kw["x_src"].rearrange("(c p) t -> p c t", p=P)
                    rdst = kw["x_dst"].rearrange("(c p) t -> p c t", p=P)

                items = [(nb_, ti) for nb_ in range(NBK) for ti in range(NQX)]

                def ldw(nb_):
                    for w in range(nW):
                        S.dma(POOL, wb[w][nb_ % 2][:], wsrc[w][:, :, nb_ * nbw:(nb_ + 1) * nbw], wbt[w][nb_ % 2], writes=[wbt[w][nb_ % 2]])

                def ldx(ix):
                    nb_, ti = items[ix]
                    S.dma(SP, xt[ix % 3][:], xsrc[:, :, ti * TX:(ti + 1) * TX], xtt[ix % 3], writes=[xtt[ix % 3]])
                    if mode == "resid":
                        S.dma(SP, xi[ix % 3][:], rsrc[:, nb_ * NJ:(nb_ + 1) * NJ, ti * TX:(ti + 1) * TX], xit[ix % 3], writes=[xit[ix % 3]])
                    if mode == "glu" and kw.get("gate_e") is not None:
                        S.dma(SP, gbx[ix % 3][:], gatesB[kw["gate_e"], :, ti * TX:(ti + 1) * TX], gbxt[ix % 3], writes=[gbxt[ix % 3]])
                ldw(0)
                ldx(0)
                if len(items) > 1:
                    ldx(1)
                pk = 0
                for ix, (nb_, ti) in enumerate(items):
                    if ti == 0 and nb_ + 1 < NBK:
                        ldw(nb_ + 1)
                    if ix + 2 < len(items):
                        ldx(ix + 2)
                    X, tX = xt[ix % 3], xtt[ix % 3]
                    Wb = [wb[w][nb_ % 2] for w in range(nW)]
                    tW = [wbt[w][nb_ % 2] for w in range(nW)]
                    if mode == "tm":
                        O, tO = ot[ix % 2], ott[ix % 2]
                        for s in range(TX // P):
                            pb = pk % 8
                            pk += 1
                            for kc in range(KCk):
                                S.mm(pst[pb], ps[:, pb, 0:nbw], tX, X[:, kc, s * P:(s + 1) * P], tW[0], Wb[0][:, kc, :], kc == 0, kc == KCk - 1)
                            S.cp(ACT if s % 2 == 0 else DVE, tO, O[:, s, :], pst[pb], ps[:, pb, 0:nbw])
                        c0 = kw["dst_col"] + nb_ * nbw
                        S.dma(SP, vtm[ti * TX:(ti + 1) * TX, c0:c0 + nbw].rearrange("(s p) n -> p s n", p=P), O[:], tO, reads=[tO])
                        continue
                    if mode in ("plain", "qknorm", "glu"):
                        O, tO = og[ix % 2], ogt[ix % 2]
                    if mode == "resid":
                        O, tO = xo[ix % 2], xot[ix % 2]
                    for jn in range(NJ):
                        pbs = []
                        for w in range(nW):
                            pb = pk % 6
                            pk += 1
                            pbs.append(pb)
                            for kc in range(KCk):
                                S.mm(pst[pb], ps[:, pb, 0:TX], tW[w], Wb[w][:, kc, jn * P:(jn + 1) * P], tX, X[:, kc, :], kc == 0, kc == KCk - 1)
                        pb = pbs[0]
                        if mode == "plain":
                            S.cp(ACT if jn % 2 == 0 else DVE, tO, O[:, jn, :], pst[pb], ps[:, pb, 0:TX])
                        elif mode == "qknorm":
                            q2, tq2 = sq[jn % 2], sqt[jn % 2]
                            r2, tr2 = rs[jn % 2], rst[jn % 2]
                            S.act(tq2, q2[:], pst[pb], ps[:, pb, 0:TX], AF.Square)
                            pb2 = 6 + (jn % 2)
                            S.mm(pst[pb2], ps[:, pb2, 0:TX], t_const, CB("ones128"), tq2, q2[:], True, True)
                            S.act(tr2, r2[:], pst[pb2], ps[:, pb2, 0:TX], AF.Ln, bias=EPS)
                            S.act(tr2, r2[:], tr2, r2[:], AF.Exp, scale=-0.5)
                            S.stt(tO, O[:, jn, :], pst[pb], ps[:, pb, 0:TX], kw["gcol"], tr2, r2[:], ALU.mult, ALU.mult, extra_reads=[t_small])
                        elif mode == "glu":
                            g2, tg2 = sg[jn % 2], sgt[jn % 2]
                            S.act(tg2, g2[:], pst[pbs[0]], ps[:, pbs[0], 0:TX], AF.Silu)
                            if kw.get("gate_e") is not None:
                                S.tt(DVE, tg2, g2[:], tg2, g2[:], pst[pbs[1]], ps[:, pbs[1], 0:TX], ALU.mult)
                                S.tt(POOL, tO, O[:, jn, :], tg2, g2[:], gbxt[ix % 3], gbx[ix % 3][:], ALU.mult)
                            else:
                                S.tt(DVE, tO, O[:, jn, :], tg2, g2[:], pst[pbs[1]], ps[:, pbs[1], 0:TX], ALU.mult)
                        elif mode == "resid":
                            cj = nb_ * NJ + jn
                            S.stt(tO, O[:, jn, :], pst[pb], ps[:, pb, 0:TX], kw["gcol"][:, cj:cj + 1], xit[ix % 3], xi[ix % 3][:, jn, :],
                                  ALU.mult, ALU.add, extra_reads=[t_ada])
                    if mode == "resid":
                        S.dma(SP, rdst[:, nb_ * NJ:(nb_ + 1) * NJ, ti * TX:(ti + 1) * TX], O[:], tO, reads=[tO])
                    else:
                        r0 = kw["dst_row"] + nb_ * nbw
                        dd = kw["dst"][r0:r0 + nbw, ti * TX:(ti + 1) * TX].rearrange("(c p) t -> p c t", p=P)
                        S.dma(SP, dd, O[:], tO, reads=[tO])
                S.end_phase(LBL[0] if "linear" == "linear" else "linear")

        def phase_down(x_dram, K, W, N, x_src, x_dst, gcol):
            KCk = K // P
            assert KCk % 2 == 0
            KH = KCk // 2
            nbw, NJ = 256, 2
            NBK = N // nbw
            LBL[0] = "down_K%d_N%d" % (K, N)
            with contextlib.ExitStack() as st:
                wb = [sbt(st, "dw%d" % i, [P, KCk, nbw], BF16) for i in range(2)]
                wbt = [S.t() for _ in range(2)]
                xt = [sbt(st, "dx%d" % i, [P, KH, TT], BF16) for i in range(3)]
                xtt = [S.t() for _ in range(3)]
                xi = [sbt(st, "dxi%d" % i, [P, NJ, TT], F32) for i in range(3)]
                xit = [S.t() for _ in range(3)]
                xo = [sbt(st, "dxo%d" % i, [P, NJ, TT], F32) for i in range(2)]
                xot = [S.t() for _ in range(2)]
                ps = pst_(st, "ps_d", [P, 8, TT], F32)
                pst = [S.t() for _ in range(8)]
                xsrc = x_dram.rearrange("(kc p) t -> p kc t", p=P)
                wsrc = W.rearrange("(kc p) n -> p kc n", p=P)
                rsrc = x_src.rearrange("(c p) t -> p c t", p=P)
                rdst = x_dst.rearrange("(c p) t -> p c t", p=P)
                parts = [(nb_, ti, hf) for nb_ in range(NBK) for ti in range(NQT) for hf in range(2)]

                def ldw(nb_):
                    S.dma(POOL, wb[nb_ % 2][:], wsrc[:, :, nb_ * nbw:(nb_ + 1) * nbw], wbt[nb_ % 2], writes=[wbt[nb_ % 2]])

                def ldx(px):
                    nb_, ti, hf = parts[px]
                    S.dma(SP, xt[px % 3][:], xsrc[:, hf * KH:(hf + 1) * KH, ti * TT:(ti + 1) * TT], xtt[px % 3], writes=[xtt[px % 3]])
                    if hf == 0:
                        ix = px // 2
                        S.dma(SP, xi[ix % 3][:], rsrc[:, nb_ * NJ:(nb_ + 1) * NJ, ti * TT:(ti + 1) * TT], xit[ix % 3], writes=[xit[ix % 3]])
                ldw(0)
                ldx(0)
                ldx(1)
                for px, (nb_, ti, hf) in enumerate(parts):
                    ix = px // 2
                    if ti == 0 and hf == 0 and nb_ + 1 < NBK:
                        ldw(nb_ + 1)
                    if px + 2 < len(parts):
                        ldx(px + 2)
                    X, tX = xt[px % 3], xtt[px % 3]
                    Wb, tW = wb[nb_ % 2], wbt[nb_ % 2]
                    for jn in range(NJ):
                        pb = (ix % 4) * 2 + jn
                        for k in range(KH):
                            kc = hf * KH + k
                            S.mm(pst[pb], ps[:, pb, :], tW, Wb[:, kc, jn * P:(jn + 1) * P], tX, X[:, k, :], kc == 0, kc == KCk - 1)
                    if hf == 1:
                        O, tO = xo[ix % 2], xot[ix % 2]
                        for jn in range(NJ):
                            pb = (ix % 4) * 2 + jn
                            cj = nb_ * NJ + jn
                            S.stt(tO, O[:, jn, :], pst[pb], ps[:, pb, :], gcol[:, cj:cj + 1], xit[ix % 3], xi[ix % 3][:, jn, :],
                                  ALU.mult, ALU.add, extra_reads=[t_ada])
                        S.dma(SP, rdst[:, nb_ * NJ:(nb_ + 1) * NJ, ti * TT:(ti + 1) * TT], O[:], tO, reads=[tO])
                S.end_phase(LBL[0])

        def phase_softattn(kind, l_idx):
            if kind == "diff":
                NH, EW = cfg.AH, 256
                lam_init = 0.8 - 0.6 * math.exp(-0.3 * l_idx)
            else:
                NH, EW = cfg.CH, 128
            EA = EW + 1
            with contextlib.ExitStack() as st:
                nmap = 2 if kind == "diff" else 1
                kq = [[sbt(st, "ak%d_%d" % (m, i), [P, 2, T], BF16) for i in range(2)] for m in range(nmap)]
                kqt = [[S.t() for _ in range(2)] for _ in range(nmap)]
                va = [sbt(st, "av%d" % i, [P, NCK, EA], BF16) for i in range(2)]
                vat = [S.t() for _ in range(2)]
                pT = [sbt(st, "ap%d" % i, [P, TT], BF16) for i in range(3)]
                pTt = [S.t() for _ in range(3)]
                if kind == "dil":
                    pe_ = [sbt(st, "ape%d" % i, [P, TT], BF16) for i in range(2)]
                    pet = [S.t() for _ in range(2)]
                on = sbt(st, "aon", [P, 2, 4, EW], F32)
                t_on = [[S.t() for _ in range(4)] for _ in range(2)]
                rl = sbt(st, "arl", [P, 16], F32)
                t_rl = S.t()
                ob_ = [sbt(st, "aob%d" % i, [P, EW], BF16) for i in range(2)]
                obt = [S.t() for _ in range(2)]
                jk = sbt(st, "ajk", [P, EW], F32)
                t_jk = S.t()
                mo = [sbt(st, "amo%d" % i, [P, EW // P, TT], BF16) for i in range(2)]
                mot = [S.t() for _ in range(2)]
                ps = pst_(st, "ps_a", [P, 7, TT], F32)
                pst = [S.t() for _ in range(7)]
                psb = pst_(st, "ps_ab", [P, 4, P], BF16)
                psbt = [S.t()] * 4
                if kind == "dil":
                    cb2s = sbt(st, "cb2s", ab2.shape, BF16)
                    t_c2 = S.t()
                    S.dma(SP, cb2s[:], cb2_in, t_c2, writes=[t_c2])

                    def CB2(name, rows=slice(0, P)):
                        o, w = ob2[name]
                        return cb2s[rows, o:o + w]
                misc = smallp[:, o_misc:o_misc + 16]
                hg = smallp[:, o_hg:o_hg + 256]
                if kind == "diff":
                    j = l_idx // 2
                    S.dma(SP, misc[:, 0:4], alam[j], t_small, writes=[t_small])
                    S.dma(SP, hg, ahg[j], t_small, writes=[t_small])
                    S.tt(DVE, t_small, misc[:, 4:5], t_small, misc[:, 0:1], t_small, misc[:, 1:2], ALU.mult)
                    S.tt(DVE, t_small, misc[:, 5:6], t_small, misc[:, 2:3], t_small, misc[:, 3:4], ALU.mult)
                    S.mm(pst[6], ps[:, 6, 0:2], t_const, CF("onesf"), t_small, misc[:, 4:6], True, True)
                    S.act(t_small, misc[:, 6:8], pst[6], ps[:, 6, 0:2], AF.Exp)
                    S.tt(DVE, t_small, misc[:, 8:9], t_small, misc[:, 6:7], t_small, misc[:, 7:8], ALU.subtract)
                    S.ts(DVE, t_small, misc[:, 9:10], t_small, misc[:, 8:9], lam_init, -1.0, ALU.add, ALU.mult)
                    S.ts(DVE, t_small, hg, t_small, hg, 1.0 - lam_init, None, ALU.mult)
                    neglam = misc[:, 9:10]

                def load_head(h):
                    i = h % 2
                    if kind == "diff":
                        rows = [(cfg.AW + (2 * h + m) * P, (2 * h + m) * P) for m in range(2)]
                        vcol = h * 256
                    else:
                        rows = [(cfg.CW + h * P, h * P)]
                        vcol = h * P
                    for m in range(nmap):
                        S.dma(SP, kq[m][i][:, 0, :], qkT[rows[m][0]:rows[m][0] + P, :], kqt[m][i], writes=[kqt[m][i]])
                        S.dma(SP, kq[m][i][:, 1, :], qkT[rows[m][1]:rows[m][1] + P, :], kqt[m][i], writes=[kqt[m][i]])
                    S.dma(SP, va[i][:, :, 0:EW], vtm[:, vcol:vcol + EW].rearrange("(c p) e -> p c e", p=P), vat[i], writes=[vat[i]])
                    S.ms(POOL, vat[i], va[i][:, :, EW:EA], 1.0)
                load_head(0)
                pT.append(sbt(st, "ap3", [P, TT], BF16))
                pTt.append(S.t())
                if kind == "dil":
                    pe_.append(sbt(st, "ape2", [P, TT], BF16))
                    pet.append(S.t())
                LA = 2
                for h in range(NH):
                    if h + 1 < NH:
                        load_head(h + 1)
                    i = h % 2
                    V, tV = va[i], vat[i]
                    jobs = []
                    for qt in range(NQT):
                        for m in range(nmap):
                            clo = 0 if kind == "diff" else max(0, 4 * qt - 16)
                            chi = 4 * qt + 3
                            for c in range(clo, chi + 1):
                                jobs.append((qt, m, c, c == chi))

                    def emit_S(ix):
                        qt, m, c, _ = jobs[ix]
                        KQ, tKQ = kq[m][i], kqt[m][i]
                        pb = 4 + (ix % 3)
                        d = c - 4 * qt
                        if kind == "diff":
                            S.mm(pst[pb], ps[:, pb, :], tKQ, KQ[:, 0, c * P:(c + 1) * P], tKQ, KQ[:, 1, qt * TT:(qt + 1) * TT], True, d < 0)
                            if d >= 0:
                                S.mm(pst[pb], ps[:, pb, :], t_const, CB("ident"), t_const, CB("mneg%d" % d), False, True)
                        else:
                            S.mm(pst[pb], ps[:, pb, :], tKQ, KQ[:, 0, c * P:(c + 1) * P], tKQ, KQ[:, 1, qt * TT:(qt + 1) * TT], True, False)
                            S.mm(pst[pb], ps[:, pb, :], t_const, CB("ones", slice(0, 1)), t_c2, CB2("shift%d" % h, slice(0, 1)), False, True)
                    for ix in range(min(LA, len(jobs))):
                        emit_S(ix)
                    for ix, (qt, m, c, lastc) in enumerate(jobs):
                        if ix + LA < len(jobs):
                            emit_S(ix + LA)
                        MO, tMO = mo[qt % 2], mot[qt % 2]
                        pb = 4 + (ix % 3)
                        d = c - 4 * qt
                        Pt, tPt = pT[ix % 4], pTt[ix % 4]
                        if kind == "diff":
                            o_, w_ = of["biasA%d" % h]
                            S.act(tPt, Pt[:], pst[pb], ps[:, pb, :], AF.Exp, bias=cfs[:, o_ + d + 31:o_ + d + 32], scale=scale, extra_reads=[t_const])
                        else:
                            Pe, tPe = pe_[ix % 3], pet[ix % 3]
                            o_, w_ = of["biasC%d" % h]
                            S.act(tPe, Pe[:], pst[pb], ps[:, pb, :], AF.Exp, bias=cfs[:, o_ + d + 16:o_ + d + 17], scale=scale, extra_reads=[t_const])
                            u = (4 * qt - c) + 3
                            S.tt(DVE if ix % 2 else POOL, tPt, Pt[:], tPe, Pe[:], t_c2, CB2("cnt%d" % u), ALU.mult)
                        for s in range(4):
                            cs = 4 * qt + s
                            if kind == "diff":
                                lo, hi = 0, cs
                            else:
                                lo, hi = max(0, cs - 16), cs
                            if c < lo or c > hi:
                                continue
                            S.mm(pst[s], ps[:, s, 0:EA], tPt, Pt[:, s * P:(s + 1) * P], tV, V[:, c, :], c == lo, c == hi)
                        if not lastc:
                            continue
                        for s in range(4):
                            S.op(DVE, lambda e, a=rl[:, m * 4 + s:m * 4 + s + 1], b_=ps[:, s, EW:EA]: e.reciprocal(out=a, in_=b_), [pst[s]], [t_rl])
                            S.ts(DVE, t_on[m][s], on[:, m, s, :], pst[s], ps[:, s, 0:EW], rl[:, m * 4 + s:m * 4 + s + 1], None, ALU.mult, extra_reads=[t_rl])
                        if m != nmap - 1:
                            continue
                        for s in range(4):
                            OB, tOB = ob_[s % 2], obt[s % 2]
                            if kind == "diff":
                                S.stt(t_on[0][s], on[:, 0, s, :], t_on[1][s], on[:, 1, s, :], neglam, t_on[0][s], on[:, 0, s, :], ALU.mult, ALU.add, extra_reads=[t_small])
                                ss = rl[:, 8 + s:9 + s]
                                S.ms(DVE, t_rl, ss, 0.0)
                                S.op(ACT, lambda e, o1=jk[:], i1=on[:, 0, s, :], a1=ss: e.activation(out=o1, in_=i1, func=AF.Square, accum_out=a1), [t_on[0][s]], [t_jk, t_rl])
                                S.act(t_rl, ss, t_rl, ss, AF.Ln, bias=EPS, scale=1.0 / 256)
                                S.act(t_rl, ss, t_rl, ss, AF.Exp, scale=-0.5)
                                S.stt(tOB, OB[:], t_on[0][s], on[:, 0, s, :], ss, t_small, hg, ALU.mult, ALU.mult, extra_reads=[t_rl])
                            else:
                                S.cp(ACT, tOB, OB[:], t_on[0][s], on[:, 0, s, :])
                            for jn in range(EW // P):
                                pbb = (s * 2 + jn) % 4
                                S.tr(psbt[pbb], psb[:, pbb, :], tOB, OB[:, jn * P:(jn + 1) * P], t_const, CB("ident"))
                                S.cp(POOL if False else DVE, tMO, MO[:, jn, s * P:(s + 1) * P], psbt[pbb], psb[:, pbb, :])
                        r0 = h * EW
                        S.dma(SP, mixT[r0:r0 + EW, qt * TT:(qt + 1) * TT].rearrange("(c p) t -> p c t", p=P), MO[:], tMO, reads=[tMO])
                S.end_phase(LBL[0] if "softattn" == "linear" else "softattn")

        def phase_sb():
            with contextlib.ExitStack() as st:
                kq = [sbt(st, "sk%d" % i, [P, 2, T], BF16) for i in range(2)]
                kqt = [S.t() for _ in range(2)]
                vv = [sbt(st, "sv%d" % i, [P, NCK, P], BF16) for i in range(2)]
                vvt = [S.t() for _ in range(2)]
                E = [sbt(st, "sE%d" % i, [P, TT], F32) for i in range(3)]
                Et = [S.t() for _ in range(3)]
                Lb = [sbt(st, "sL%d" % i, [P, TT], BF16) for i in range(3)]
                Lt = [S.t() for _ in range(3)]
                R = [sbt(st, "sR%d" % i, [P, TT], F32) for i in range(2)]
                Rt = [S.t() for _ in range(2)]
                AG = [sbt(st, "sA%d" % i, [P, TT], F32) for i in range(3)]
                AGt = [S.t() for _ in range(3)]
                Wt = [sbt(st, "sW%d" % i, [P, TT], BF16) for i in range(3)]
                Wtt = [S.t() for _ in range(3)]
                mo = [sbt(st, "smo%d" % i, [P, TT], BF16) for i in range(2)]
                mot = [S.t() for _ in range(2)]
                ps = pst_(st, "ps_s", [P, 8, TT], F32)
                pst = [S.t() for _ in range(8)]

                def load_head(h):
                    i = h % 2
                    rq = 2 * cfg.AW + h * P
                    rk = 2 * cfg.AW + cfg.BW + h * P
                    S.dma(SP, kq[i][:, 0, :], qkT[rk:rk + P, :], kqt[i], writes=[kqt[i]])
                    S.dma(SP, kq[i][:, 1, :], qkT[rq:rq + P, :], kqt[i], writes=[kqt[i]])
                    vc = cfg.AW + h * P
                    S.dma(SP, vv[i][:], vtm[:, vc:vc + P].rearrange("(c p) e -> p c e", p=P), vvt[i], writes=[vvt[i]])
                load_head(0)
                ri = [0]
                for h in range(cfg.BH):
                    if h + 1 < cfg.BH:
                        load_head(h + 1)
                    i = h % 2
                    KQ, tKQ, V, tV = kq[i], kqt[i], vv[i], vvt[i]
                    jobs = []
                    for qt in range(NQT):
                        chi = 4 * qt + 3
                        for c in range(chi, -1, -1):
                            jobs.append((qt, c, c == chi))

                    def stage_A(ix):
                        qt, c, first = jobs[ix]
                        b2, b3 = ix % 2, ix % 3
                        d = c - 4 * qt
                        S.mm(pst[b2], ps[:, b2, :], tKQ, KQ[:, 0, c * P:(c + 1) * P], tKQ, KQ[:, 1, qt * TT:(qt + 1) * TT], True, True)
                        S.act(Et[b3], E[b3][:], pst[b2], ps[:, b2, :], AF.Exp, scale=scale)
                        if d >= 0:
                            S.tt(POOL, Et[b3], E[b3][:], Et[b3], E[b3][:], t_const, CF("m01%d" % d), ALU.mult)
                        S.act(Lt[b3], Lb[b3][:], Et[b3], E[b3][:], AF.Ln, bias=1.0)

                    def stage_B(ix):
                        qt, c, first = jobs[ix]
                        b2, b3 = ix % 2, ix % 3
                        S.mm(pst[2 + b2], ps[:, 2 + b2, :], t_const, CB("tri"), Lt[b3], Lb[b3][:], True, True)
                        if c > 0:
                            S.mm(pst[4 + b2], ps[:, 4 + b2, :], t_const, CB("ones"), Lt[b3], Lb[b3][:], True, True)
                        if first:
                            S.act(AGt[b3], AG[b3][:], pst[2 + b2], ps[:, 2 + b2, :], AF.Exp, scale=-1.0)
                        else:
                            S.tt(DVE, AGt[b3], AG[b3][:], pst[2 + b2], ps[:, 2 + b2, :], Rt[ri[0] % 2], R[ri[0] % 2][:], ALU.add)
                            S.act(AGt[b3], AG[b3][:], AGt[b3], AG[b3][:], AF.Exp, scale=-1.0)
                        if c > 0:
                            if first:
                                S.cp(DVE, Rt[(ri[0] + 1) % 2], R[(ri[0] + 1) % 2][:], pst[4 + b2], ps[:, 4 + b2, :])
                            else:
                                S.tt(DVE, Rt[(ri[0] + 1) % 2], R[(ri[0] + 1) % 2][:], pst[4 + b2], ps[:, 4 + b2, :], Rt[ri[0] % 2], R[ri[0] % 2][:], ALU.add)
                            ri[0] += 1
                        S.tt(POOL, Wtt[b3], Wt[b3][:], Et[b3], E[b3][:], AGt[b3], AG[b3][:], ALU.mult)

                    def stage_C(ix):
                        qt, c, first = jobs[ix]
                        b3 = ix % 3
                        po = 6 + (qt % 2)
                        S.mm(pst[po], ps[:, po, :], tV, V[:, c, :], Wtt[b3], Wt[b3][:], first, c == 0)
                        if c == 0:
                            MO, tMO = mo[qt % 2], mot[qt % 2]
                            S.cp(ACT, tMO, MO[:], pst[po], ps[:, po, :])
                            r0 = cfg.AW + h * P
                            S.dma(SP, mixT[r0:r0 + P, qt * TT:(qt + 1) * TT], MO[:], tMO, reads=[tMO])
                    n = len(jobs)
                    stage_A(0)
                    if n > 1:
                        stage_A(1)
                    stage_B(0)
                    for ix in range(n):
                        if ix + 2 < n:
                            stage_A(ix + 2)
                        if ix + 1 < n:
                            stage_B(ix + 1)
                        stage_C(ix)
                S.end_phase(LBL[0] if "sb" == "linear" else "sb")

        def phase_conv(j):
            HT = T // 2
            with contextlib.ExitStack() as st:
                cw = smallp[:, o_cw:o_cw + KD * 34]
                S.dma(SP, cw[:, 0:KD * 31], convw[j], t_small, writes=[t_small])
                S.dma(SP, cw[:, KD * 31:KD * 32], convb[j], t_small, writes=[t_small])
                S.dma(SP, cw[:, KD * 32:KD * 33], lng[j], t_small, writes=[t_small])
                S.dma(SP, cw[:, KD * 33:KD * 34], lnb[j], t_small, writes=[t_small])
                ag = [sbt(st, "cag%d" % i, [P, 2, T], BF16) for i in range(2)]
                agt = [S.t() for _ in range(2)]
                sig = sbt(st, "csig", [P, T], F32)
                t_sig = S.t()
                hp = sbt(st, "chp", [P, 32 + T], F32)
                t_hp = S.t()
                yy = [sbt(st, "cy%d" % i, [P, T], F32) for i in range(2)]
                yyt = [[S.t(), S.t()] for _ in range(2)]
                yb = sbt(st, "cyb", [P, 2, TT], BF16)
                t_yb = S.t()
                sS = sbt(st, "csS", [P, T], F32)
                sQ = sbt(st, "csQ", [P, T], F32)
                t_sS = [S.t() for _ in range(NQT)]
                t_sQ = [S.t() for _ in range(NQT)]
                ps = pst_(st, "ps_c", [P, 4, TT], F32)
                pst = [S.t() for _ in range(4)]
                r_a = 2 * cfg.CW
                r_g = 2 * cfg.CW + cfg.DCH

                def ld(cc):
                    S.dma(SP, ag[cc % 2][:, 0, :], qkT[r_a + cc * P:r_a + (cc + 1) * P, :], agt[cc % 2], writes=[agt[cc % 2]])
                    S.dma(SP, ag[cc % 2][:, 1, :], qkT[r_g + cc * P:r_g + (cc + 1) * P, :], agt[cc % 2], writes=[agt[cc % 2]])
                ld(0)
                S.ms(DVE, t_hp, hp[:, 0:32], 0.0)
                if CONV_PE:
                    hpb = sbt(st, "chpb", [P, 32 + T], BF16)
                    t_hpb = S.t()
                    S.ms(DVE, t_hpb, hpb[:, 0:32], 0.0)
                    dgm = [sbt(st, "cdg%d" % i, [P, 31, P], BF16) for i in range(2)]
                    dgmt = [S.t() for _ in range(2)]
                for cc in range(KD):
                    if cc + 1 < KD:
                        ld(cc + 1)
                    A, tA = ag[cc % 2], agt[cc % 2]
                    Y, tY = yy[cc % 2], yyt[cc % 2]
                    S.act(t_sig, sig[:], tA, A[:, 1, :], AF.Sigmoid)
                    S.tt(DVE, t_hp, hp[:, 32:32 + T], tA, A[:, 0, :], t_sig, sig[:], ALU.mult)
                    if CONV_PE:
                        Dg, tDg = dgm[cc % 2], dgmt[cc % 2]
                        for tap in range(31):
                            wcol = cw[:, cc * 31 + tap: cc * 31 + tap + 1]
                            S.ts(DVE, tDg, Dg[:, tap, :], t_const, CB("ident"), wcol, None, ALU.mult, extra_reads=[t_small])
                        S.cp(POOL, t_hpb, hpb[:, 32:32 + T], t_hp, hp[:, 32:32 + T])
                        for ti in range(NQT):
                            pb = 2 + (ti % 2)
                            for tap in range(31):
                                S.mm(pst[pb], ps[:, pb, :], tDg, Dg[:, tap, :], t_hpb, hpb[:, 2 + tap + ti * TT: 2 + tap + (ti + 1) * TT], tap == 0, tap == 30)
                            S.act(tY[0], Y[:, ti * TT:(ti + 1) * TT], pst[pb], ps[:, pb, :], AF.Identity, bias=cw[:, KD * 31 + cc:KD * 31 + cc + 1], extra_reads=[t_small])
                    else:
                        for tap in range(31):
                            src = hp[:, 2 + tap: 2 + tap + T]
                            wcol = cw[:, cc * 31 + tap: cc * 31 + tap + 1]
                            if tap == 0:
                                S.ts(DVE, tY[0], Y[:], t_hp, src, wcol, cw[:, KD * 31 + cc:KD * 31 + cc + 1], ALU.mult, ALU.add, extra_reads=[t_small])
                            else:
                                S.stt(tY[0], Y[:], t_hp, src, wcol, tY[0], Y[:], ALU.mult, ALU.add, extra_reads=[t_small])
                    for ti in range(NQT):
                        half = 0
                        ysl = Y[:, ti * TT:(ti + 1) * TT]
                        S.cp(ACT, t_yb, yb[:, 0, :], tY[half], ysl)
                        S.act(t_yb, yb[:, 1, :], tY[half], ysl, AF.Square)
                        S.mm(pst[0], ps[:, 0, :], t_const, CB("onesDCH"), t_yb, yb[:, 0, :], True, True)
                        S.mm(pst[1], ps[:, 1, :], t_const, CB("onesDCH"), t_yb, yb[:, 1, :], True, True)
                        if cc == 0:
                            S.cp(DVE, t_sS[ti], sS[:, ti * TT:(ti + 1) * TT], pst[0], ps[:, 0, :])
                            S.cp(DVE, t_sQ[ti], sQ[:, ti * TT:(ti + 1) * TT], pst[1], ps[:, 1, :])
                        else:
                            S.tt(DVE, t_sS[ti], sS[:, ti * TT:(ti + 1) * TT], pst[0], ps[:, 0, :], t_sS[ti], sS[:, ti * TT:(ti + 1) * TT], ALU.add)
                            S.tt(DVE, t_sQ[ti], sQ[:, ti * TT:(ti + 1) * TT], pst[1], ps[:, 1, :], t_sQ[ti], sQ[:, ti * TT:(ti + 1) * TT], ALU.add)
                    S.dma(SP, ycv[cc * P:(cc + 1) * P, :], Y[:], tY[0], reads=[tY[0]])
                for ti in range(NQT):
                    sl = slice(ti * TT, (ti + 1) * TT)
                    S.tt(DVE, t_sig, sig[:, sl], t_sS[ti], sS[:, sl], t_sS[ti], sS[:, sl], ALU.mult)
                    S.tt(DVE, t_sQ[ti], sQ[:, sl], t_sQ[ti], sQ[:, sl], t_sig, sig[:, sl], ALU.subtract)
                    S.act(t_sQ[ti], sQ[:, sl], t_sQ[ti], sQ[:, sl], AF.Ln, bias=EPS)
                    S.act(t_sQ[ti], sQ[:, sl], t_sQ[ti], sQ[:, sl], AF.Exp, scale=-0.5)
                S.end_phase(LBL[0] if "conv" == "linear" else "conv")
                ob2 = [ag[i][:, 0, :] for i in range(2)]
                ob2t = [S.t() for _ in range(2)]
                t_y2 = [S.t() for _ in range(2)]
                t_st = S.t()

                def ld2(cc):
                    S.dma(SP, yy[cc % 2][:], ycv[cc * P:(cc + 1) * P, :], t_y2[cc % 2], writes=[t_y2[cc % 2]])
                ld2(0)
                for cc in range(KD):
                    if cc + 1 < KD:
                        ld2(cc + 1)
                    Y, tY = yy[cc % 2], t_y2[cc % 2]
                    S.tt(DVE, tY, Y[:], tY, Y[:], t_st, sS[:], ALU.subtract)
                    S.tt(POOL, tY, Y[:], tY, Y[:], t_st, sQ[:], ALU.mult)
                    S.act(ob2t[cc % 2], ob2[cc % 2], tY, Y[:], AF.Silu, bias=cw[:, KD * 33 + cc:KD * 33 + cc + 1],
                          scale=cw[:, KD * 32 + cc:KD * 32 + cc + 1], extra_reads=[t_small])
                    r0 = cfg.CW + cc * P
                    S.dma(SP, mixT[r0:r0 + P, :], ob2[cc % 2], ob2t[cc % 2], reads=[ob2t[cc % 2]])
                S.end_phase(LBL[0] if "conv" == "linear" else "conv")

        def down(x_dram, K, W, N, x_src, x_dst, gcol):
            kct = int(_os.environ.get("KDBG_KCT", "16"))
            if K // P > kct:
                phase_down(x_dram, K, W, N, x_src, x_dst, gcol)
            else:
                phase_linear(x_dram, K, [W], N, "resid", x_src=x_src, x_dst=x_dst, gcol=gcol)

        for l in layers:
            j = l // 2
            phase_ada(l)
            first = (l == layers[0])
            lastl = (l == layers[-1])
            for b in range(NB):
                x0 = xT_in[b] if first else xres[b]
                fin = yT_out[b] if lastl else xres[b]
                phase_norm(b, x0, 0)
                qg = smallp[:, o_misc + 10:o_misc + 11]
                kg = smallp[:, o_misc + 11:o_misc + 12]
                if l % 2 == 0:
                    S.dma(SP, qg, aqg[j], t_small, writes=[t_small])
                    S.dma(SP, kg, akg[j], t_small, writes=[t_small])
                    W = ev_w_in[j]
                    AW, BW = cfg.AW, cfg.BW
                    phase_linear(hT, D, [W[:, 0:AW]], AW, "qknorm", gcol=qg, dst=qkT, dst_row=0)
                    phase_linear(hT, D, [W[:, AW:2 * AW]], AW, "qknorm", gcol=kg, dst=qkT, dst_row=AW)
                    phase_linear(hT, D, [W[:, 2 * AW:3 * AW]], AW, "tm", dst_col=0)
                    phase_linear(hT, D, [W[:, 3 * AW:3 * AW + 2 * BW]], 2 * BW, "plain", dst=qkT, dst_row=2 * AW)
                    phase_linear(hT, D, [W[:, 3 * AW + 2 * BW:3 * AW + 3 * BW]], BW, "tm", dst_col=AW)
                    phase_softattn("diff", l)
                    phase_sb()
                    wout = ev_w_out[j]
                else:
                    S.dma(SP, qg, cqg[j], t_small, writes=[t_small])
                    S.dma(SP, kg, ckg[j], t_small, writes=[t_small])
                    W = od_w_in[j]
                    CW = cfg.CW
                    phase_linear(hT, D, [W[:, 0:CW]], CW, "qknorm", gcol=qg, dst=qkT, dst_row=0)
                    phase_linear(hT, D, [W[:, CW:2 * CW]], CW, "qknorm", gcol=kg, dst=qkT, dst_row=CW)
                    phase_linear(hT, D, [W[:, 2 * CW:3 * CW]], CW, "tm", dst_col=0)
                    phase_linear(hT, D, [W[:, 3 * CW:3 * CW + 2 * cfg.DCH]], 2 * cfg.DCH, "plain", dst=qkT, dst_row=2 * CW)
                    phase_softattn("dil", l)
                    phase_conv(j)
                    wout = od_w_out[j]
                phase_linear(mixT, D, [wout], D, "resid", x_src=x0, x_dst=xb, gcol=ada_col(b, 2))
                if l % 2 == 0:
                    phase_norm(b, xb, 1)
                    phase_linear(hT, D, [ffn_wg[j], ffn_wu[j]], DFF, "glu", dst=AT, dst_row=0)
                    down(AT, DFF, ffn_wd[j], D, xb, fin, ada_col(b, 5))
                else:
                    phase_norm(b, xb, 1, router_l=j)
                    cur, other = xb, xc
                    for ex in range(NE):
                        phase_linear(hT, D, [moe_wg[j, ex], moe_wu[j, ex]], DFF, "glu", dst=AT, dst_row=0, gate_e=ex)
                        dst = fin if ex == NE - 1 else other
                        down(AT, DFF, moe_wd[j, ex], D, cur, dst, ada_col(b, 5))
                        cur, other = dst, cur
    build_program.stats = S.stats
    return nc, (ab, af, ab2)


def _colmajor(v, kc):
    v = np.asarray(v, np.float32)
    lead = v.shape[:-1]
    return np.ascontiguousarray(np.swapaxes(v.reshape(*lead, kc, P), -1, -2))


def make_in_maps(cfg, inp, consts):
    ab, af, ab2 = consts
    D, T, NB, KC, NE = cfg.D, cfg.T, cfg.NB, cfg.KC, cfg.NE
    KD = cfg.DCH // P
    f = lambda a: np.ascontiguousarray(np.asarray(a, np.float32))
    shared = {
        "ada_w": f(inp["ada_w"]),
        "ada_bT": _colmajor(inp["ada_b"], 6 * KC),
        "nmgT": _colmajor(inp["norm_mix_g"], KC),
        "nfgT": _colmajor(inp["norm_ffn_g"], KC),
        "ev_w_in": f(inp["ev_w_in"]), "ev_w_out": f(inp["ev_w_out"]),
        "aqg": f(np.asarray(inp["a_q_norm_g"])[:, :, None]), "akg": f(np.asarray(inp["a_k_norm_g"])[:, :, None]),
        "alam": f(np.transpose(np.asarray(inp["a_lambda"]), (0, 2, 1))),
        "ahg": f(np.broadcast_to(np.asarray(inp["a_head_norm_g"])[:, None, :], (cfg.NEV, P, 256))),
        "ffn_wg": f(inp["ffn_w_gate"]), "ffn_wu": f(inp["ffn_w_up"]), "ffn_wd": f(inp["ffn_w_down"]),
        "od_w_in": f(inp["od_w_in"]), "od_w_out": f(inp["od_w_out"]),
        "cqg": f(np.asarray(inp["c_q_norm_g"])[:, :, None]), "ckg": f(np.asarray(inp["c_k_norm_g"])[:, :, None]),
        "convw": f(np.transpose(np.asarray(inp["d_conv_w"]).reshape(cfg.NOD, 31, KD, P), (0, 3, 2, 1)).reshape(cfg.NOD, P, KD * 31)),
        "convb": _colmajor(inp["d_conv_b"], KD), "lng": _colmajor(inp["d_ln_g"], KD), "lnb": _colmajor(inp["d_ln_b"], KD),
        "routT": f(np.transpose(np.asarray(inp["moe_router"]).reshape(cfg.NOD, KC, P, NE), (0, 2, 1, 3)).reshape(cfg.NOD, P, KC * NE)),
        "moe_wg": f(inp["moe_w_gate"]), "moe_wu": f(inp["moe_w_up"]), "moe_wd": f(inp["moe_w_down"]),
        "cb": ab, "cf": af, "cb2": ab2,
    }
    x = np.asarray(inp["x"], np.float32)
    c = np.asarray(inp["c"], np.float32)
    maps = []
    for core in range(cfg.ncores):
        bs = range(core * NB, (core + 1) * NB)
        m = dict(shared)
        m["xT"] = np.ascontiguousarray(np.stack([x[b].T for b in bs]))
        cc = np.stack([c[b].reshape(KC, P).T for b in bs], axis=1)
        m["cT"] = np.ascontiguousarray(cc.reshape(P, NB * KC))
        maps.append(m)
    return maps


def run(cfg, inp, layers=None):
    nc, consts = build_program(cfg, layers)
    maps = make_in_maps(cfg, inp, consts)
    res = run_bass_kernel_spmd(nc, maps, core_ids=list(range(cfg.ncores)))
    outs = []
    for core in range(cfg.ncores):
        yT = res.results[core]["yT"]
        for b in range(cfg.NB):
            outs.append(np.ascontiguousarray(yT[b].T))
    return np.stack(outs).astype(np.float32)


NCORES = 8


def kernel(**inputs):
    cfg = Cfg(D=2048, T=4096, NB=8 // NCORES, L=4, NE=8, ncores=NCORES)
    return run(cfg, inputs)
```
